# Optimizing a Trainium2 kernel written in Bass

```python
import math
import jax, jax.numpy as jnp
from jax import lax
import numpy as np

D_MODEL = 1024
BATCH = 8
SEQ = 4096
DEPTH = 1

GRID_W = 64
CTX_LEN = 256
N_HEADS = 8
HEAD_DIM = 64
V_DIM = 2 * HEAD_DIM
ATTN_WIDTH = N_HEADS * V_DIM
QK_WIDTH = N_HEADS * 2 * HEAD_DIM
D_CONV = D_MODEL
CONV_WIDTH = 31
N_EXPERTS = 16
CAPACITY_FACTOR = 2
D_FF_EXPERT = 2816
ROPE_THETA = 10000.0
Q_BLOCK = 128
EPS = 1e-6
N_MOD = 6
PROJ_WIDTH = 2 * D_CONV + 2 * QK_WIDTH + ATTN_WIDTH + 2 * D_MODEL

kernel_name = "hybrid_conv_diffattn_ecmoe_dit_layer"


def rmsnorm(x, g):
    xf = x.astype(jnp.float32)
    y = xf * lax.rsqrt(jnp.mean(xf * xf, axis=-1, keepdims=True) + EPS)
    return (y * g.astype(jnp.float32)).astype(x.dtype)


def layernorm(x, g, b):
    xf = x.astype(jnp.float32)
    mu = jnp.mean(xf, axis=-1, keepdims=True)
    xc = xf - mu
    y = xc * lax.rsqrt(jnp.mean(xc * xc, axis=-1, keepdims=True) + EPS)
    return (y * g.astype(jnp.float32) + b.astype(jnp.float32)).astype(x.dtype)


def modulate(h, shift, scale):
    return h * (1 + scale) + shift


def axial_rope_tables(n_tokens):
    rows = n_tokens // GRID_W
    row = jnp.repeat(jnp.arange(rows), GRID_W)
    col = jnp.tile(jnp.arange(GRID_W), rows)
    n_freq = HEAD_DIM // 4
    inv = ROPE_THETA ** (-jnp.arange(n_freq, dtype=jnp.float32) / n_freq)
    ang_r = row[:, None].astype(jnp.float32) * inv
    ang_c = col[:, None].astype(jnp.float32) * inv
    return jnp.cos(ang_r), jnp.sin(ang_r), jnp.cos(ang_c), jnp.sin(ang_c)


def rope_1d(x, cos, sin):
    x1, x2 = jnp.split(x, 2, axis=-1)
    return jnp.concatenate([x1 * cos - x2 * sin, x1 * sin + x2 * cos], axis=-1)


def apply_axial_rope(x, tables):
    cos_r, sin_r, cos_c, sin_c = [t[:, None, None, :].astype(x.dtype) for t in tables]
    xr, xc = jnp.split(x, 2, axis=-1)
    return jnp.concatenate([rope_1d(xr, cos_r, sin_r), rope_1d(xc, cos_c, sin_c)], axis=-1)


def split_projection(p):
    sizes = [D_CONV, D_CONV, QK_WIDTH, QK_WIDTH, ATTN_WIDTH, D_MODEL, D_MODEL]
    cuts = list(np.cumsum(sizes)[:-1])
    return jnp.split(p, cuts, axis=-1)


def conv_branch(a, b, w_dw, b_dw, ln_g, ln_b, w_o, b_o):
    u = a * jax.nn.sigmoid(b)
    u = lax.conv_general_dilated(
        u, w_dw[:, None, :].astype(u.dtype), window_strides=(1,),
        padding=[(CONV_WIDTH // 2, CONV_WIDTH // 2)],
        dimension_numbers=("NWC", "WIO", "NWC"), feature_group_count=D_CONV) + b_dw
    u = jax.nn.silu(layernorm(u, ln_g, ln_b))
    return u @ w_o + b_o


def diff_weights(q, k, lam):
    logits = jnp.einsum("bqhmd,bkhmd->bhmqk", q, k).astype(jnp.float32) * (HEAD_DIM ** -0.5)
    p = jax.nn.softmax(logits, axis=-1)
    return p[:, :, 0] - lam * p[:, :, 1]


def diff_attention_blocks(q, k, v, lam):
    bsz, t = q.shape[0], q.shape[1]
    n_blk = t // Q_BLOCK
    qb = q.reshape(bsz, n_blk, Q_BLOCK, N_HEADS, 2, HEAD_DIM).swapaxes(0, 1)

    def one_block(q_blk):
        w = diff_weights(q_blk, k, lam)
        return jnp.einsum("bhqk,bkhe->bqhe", w.astype(v.dtype), v)

    out = lax.map(one_block, qb)
    return out.swapaxes(0, 1).reshape(bsz, t, N_HEADS, V_DIM)


def diff_head_out(o, g_subln, lam_init, w_o):
    bsz, t = o.shape[0], o.shape[1]
    o = rmsnorm(o, g_subln) * (1.0 - lam_init)
    return o.reshape(bsz, t, ATTN_WIDTH) @ w_o


def ec_moe(h, w_router, w_g, w_u, w_d):
    bsz, t = h.shape[0], h.shape[1]
    cap = CAPACITY_FACTOR * t // N_EXPERTS
    aff = jax.nn.softmax(jnp.einsum("btd,de->bte", h, w_router).astype(jnp.float32), axis=-1)
    gates, idx = lax.top_k(aff.transpose(0, 2, 1), cap)
    bidx = jnp.arange(bsz)[:, None, None]
    xs = h[bidx, idx]
    hid = jax.nn.silu(jnp.einsum("becd,edf->becf", xs, w_g)) * jnp.einsum("becd,edf->becf", xs, w_u)
    ys = jnp.einsum("becf,efd->becd", hid, w_d) * gates[..., None].astype(h.dtype)
    return jnp.zeros_like(h).at[bidx, idx].add(ys)


def setup_inputs(seed: int = 0) -> dict:
    key = jax.random.key(seed)
    ks = jax.random.split(key, 32)
    f32 = jnp.float32

    def nrm(k, shape, scale):
        return jax.random.normal(k, shape, f32) * scale

    L = DEPTH
    return {
        "x": nrm(ks[0], (BATCH, SEQ, D_MODEL), 1.0),
        "c": nrm(ks[1], (BATCH, D_MODEL), 1.0),
        "ctx": nrm(ks[2], (BATCH, CTX_LEN, D_MODEL), 1.0),
        "c_ctx": nrm(ks[3], (D_MODEL,), 1.0),
        "w_ada": nrm(ks[4], (L, D_MODEL, N_MOD * D_MODEL), 0.5 * D_MODEL ** -0.5),
        "b_ada": nrm(ks[5], (L, N_MOD * D_MODEL), 0.02),
        "g_norm_mix": 1.0 + nrm(ks[6], (L, D_MODEL), 0.02),
        "g_norm_ffn": 1.0 + nrm(ks[7], (L, D_MODEL), 0.02),
        "w_in": nrm(ks[8], (L, D_MODEL, PROJ_WIDTH), D_MODEL ** -0.5),
        "w_dw": nrm(ks[9], (L, CONV_WIDTH, D_CONV), CONV_WIDTH ** -0.5),
        "b_dw": nrm(ks[10], (L, D_CONV), 0.02),
        "ln_g_conv": 1.0 + nrm(ks[11], (L, D_CONV), 0.02),
        "ln_b_conv": nrm(ks[12], (L, D_CONV), 0.02),
        "w_conv_out": nrm(ks[13], (L, D_CONV, D_MODEL), D_CONV ** -0.5),
        "b_conv_out": nrm(ks[14], (L, D_MODEL), 0.02),
        "lambda_q1": nrm(ks[15], (L, HEAD_DIM), 0.1),
        "lambda_k1": nrm(ks[16], (L, HEAD_DIM), 0.1),
        "lambda_q2": nrm(ks[17], (L, HEAD_DIM), 0.1),
        "lambda_k2": nrm(ks[18], (L, HEAD_DIM), 0.1),
        "g_subln": 1.0 + nrm(ks[19], (L, V_DIM), 0.02),
        "w_attn_out": nrm(ks[20], (L, ATTN_WIDTH, D_MODEL), ATTN_WIDTH ** -0.5),
        "w_out": nrm(ks[21], (L, D_MODEL, D_MODEL), D_MODEL ** -0.5),
        "w_router": nrm(ks[22], (L, D_MODEL, N_EXPERTS), D_MODEL ** -0.5),
        "w_expert_gate": nrm(ks[23], (L, N_EXPERTS, D_MODEL, D_FF_EXPERT), D_MODEL ** -0.5),
        "w_expert_up": nrm(ks[24], (L, N_EXPERTS, D_MODEL, D_FF_EXPERT), D_MODEL ** -0.5),
        "w_expert_down": nrm(ks[25], (L, N_EXPERTS, D_FF_EXPERT, D_MODEL), D_FF_EXPERT ** -0.5),
        "g_final": 1.0 + nrm(ks[26], (D_MODEL,), 0.02),
    }


def reference(x, c, ctx, c_ctx, w_ada, b_ada, g_norm_mix, g_norm_ffn, w_in, w_dw, b_dw,
              ln_g_conv, ln_b_conv, w_conv_out, b_conv_out, lambda_q1, lambda_k1, lambda_q2,
              lambda_k2, g_subln, w_attn_out, w_out, w_router, w_expert_gate, w_expert_up,
              w_expert_down, g_final):
    bsz, t = x.shape[0], x.shape[1]
    n_ctx = ctx.shape[1]
    rope = axial_rope_tables(t)

    for l in range(DEPTH):
        last = l == DEPTH - 1
        lam_init = 0.8 - 0.6 * math.exp(-0.3 * l)
        lam = (jnp.exp(jnp.sum(lambda_q1[l].astype(jnp.float32) * lambda_k1[l].astype(jnp.float32)))
               - jnp.exp(jnp.sum(lambda_q2[l].astype(jnp.float32) * lambda_k2[l].astype(jnp.float32)))
               + lam_init)

        mod_lat = (jax.nn.silu(c) @ w_ada[l] + b_ada[l])[:, None, :]
        mod_ctx = jax.nn.silu(c_ctx) @ w_ada[l] + b_ada[l]
        sh_m, sc_m, g_m, sh_f, sc_f, g_f = jnp.split(mod_lat, N_MOD, axis=-1)
        csh_m, csc_m, cg_m, csh_f, csc_f, cg_f = jnp.split(mod_ctx, N_MOD, axis=-1)

        h_lat = modulate(rmsnorm(x, g_norm_mix[l]), sh_m, sc_m)
        h_ctx = modulate(rmsnorm(ctx, g_norm_mix[l]), csh_m, csc_m)
        a_l, b_l, q_l, k_l, v_l, gc_l, ga_l = split_projection(h_lat @ w_in[l])
        a_c, b_c, q_c, k_c, v_c, gc_c, ga_c = split_projection(h_ctx @ w_in[l])

        q_l = apply_axial_rope(q_l.reshape(bsz, t, N_HEADS, 2, HEAD_DIM), rope)
        k_l = apply_axial_rope(k_l.reshape(bsz, t, N_HEADS, 2, HEAD_DIM), rope)
        v_l = v_l.reshape(bsz, t, N_HEADS, V_DIM)
        k_c = k_c.reshape(bsz, n_ctx, N_HEADS, 2, HEAD_DIM)
        v_c = v_c.reshape(bsz, n_ctx, N_HEADS, V_DIM)

        keys = jnp.concatenate([k_l, k_c], axis=1)
        vals = jnp.concatenate([v_l, v_c], axis=1)
        y_attn_l = diff_head_out(diff_attention_blocks(q_l, keys, vals, lam), g_subln[l], lam_init, w_attn_out[l])
        y_conv_l = conv_branch(a_l, b_l, w_dw[l], b_dw[l], ln_g_conv[l], ln_b_conv[l], w_conv_out[l], b_conv_out[l])
        mix_l = (jax.nn.sigmoid(ga_l) * y_attn_l + jax.nn.sigmoid(gc_l) * y_conv_l) @ w_out[l]

        if not last:
            q_c = q_c.reshape(bsz, n_ctx, N_HEADS, 2, HEAD_DIM)
            w_cc = diff_weights(q_c, k_c, lam)
            o_c = jnp.einsum("bhqk,bkhe->bqhe", w_cc.astype(v_c.dtype), v_c)
            y_attn_c = diff_head_out(o_c, g_subln[l], lam_init, w_attn_out[l])
            y_conv_c = conv_branch(a_c, b_c, w_dw[l], b_dw[l], ln_g_conv[l], ln_b_conv[l], w_conv_out[l], b_conv_out[l])
            mix_c = (jax.nn.sigmoid(ga_c) * y_attn_c + jax.nn.sigmoid(gc_c) * y_conv_c) @ w_out[l]
            ctx = ctx + cg_m * mix_c
            hf_c = modulate(rmsnorm(ctx, g_norm_ffn[l]), csh_f, csc_f)
            ctx = ctx + cg_f * ec_moe(hf_c, w_router[l], w_expert_gate[l], w_expert_up[l], w_expert_down[l])

        x = x + g_m * mix_l

        hf_l = modulate(rmsnorm(x, g_norm_ffn[l]), sh_f, sc_f)
        x = x + g_f * ec_moe(hf_l, w_router[l], w_expert_gate[l], w_expert_up[l], w_expert_down[l])

    return rmsnorm(x, g_final)
```

```python
import contextlib
import math

import numpy as np
import concourse.bass as bass
import concourse.mybir as mybir
from concourse.bass_utils import run_bass_kernel_spmd

F32 = mybir.dt.float32
BF16 = mybir.dt.bfloat16
I32 = mybir.dt.int32
AF = mybir.ActivationFunctionType
ALU = mybir.AluOpType
AX = mybir.AxisListType

D = 1024
T = 4096
TC = 256
TK = T + TC
NT = T // 128
NKT = TK // 128
NE = 16
CAP = 512
DFF = 2816
NFB = DFF // 128
EPS = 1e-6
LAM_INIT = 0.8 - 0.6 * math.exp(-0.3 * 0)
ENGINES = ("sync", "scalar", "vector", "gpsimd", "tensor")
NBIS = 30


class Op:
    __slots__ = ("eng", "fn", "is_dma", "deps", "signal", "count", "dsem", "dval", "idx")


class Sched:
    def __init__(self, nc, stack, n_dma_sems=8):
        self.nc = nc
        self.n_dma_sems = n_dma_sems
        self.esem = {e: stack.enter_context(nc.semaphore("p_" + e)) for e in ENGINES}
        self.dsem = {}
        for e in ("sync", "scalar", "gpsimd"):
            for k in range(n_dma_sems):
                self.dsem[(e, k)] = stack.enter_context(nc.semaphore("d_%s_%d" % (e, k)))
        self.ecount = {e: 0 for e in ENGINES}
        self.dma_uses = {k: 0 for k in self.dsem}
        self.dma_rr = {e: 0 for e in ENGINES}
        self._reset()

    def _reset(self):
        self.ops = {e: [] for e in ENGINES}
        self.last_w = {}
        self.readers = {}
        self.dma_last = {}

    def add(self, eng, fn, r=(), w=(), dma=False):
        op = Op()
        op.eng, op.fn, op.is_dma = eng, fn, dma
        op.deps = []
        op.signal = False
        op.count = None
        op.dsem = None
        op.dval = None
        op.idx = len(self.ops[eng])
        deps = set()
        for k in r:
            lw = self.last_w.get(k)
            if lw is not None:
                deps.add(lw)
        for k in w:
            lw = self.last_w.get(k)
            if lw is not None:
                deps.add(lw)
            for rd in self.readers.get(k, ()):
                deps.add(rd)
        for d in deps:
            if d.eng == "tensor" and eng == "tensor" and not d.is_dma and not dma:
                continue
            op.deps.append(d)
        if dma:
            k = self.dma_rr[eng]
            self.dma_rr[eng] = (k + 1) % self.n_dma_sems
            key = (eng, k)
            self.dma_uses[key] += 1
            op.dsem = key
            op.dval = 16 * self.dma_uses[key]
            prev = self.dma_last.get(key)
            if prev is not None:
                op.deps.append(prev)
            self.dma_last[key] = op
        for k in r:
            self.readers.setdefault(k, []).append(op)
        for k in w:
            self.last_w[k] = op
            self.readers[k] = []
        self.ops[eng].append(op)
        return op

    def emit(self):
        nc = self.nc
        for e in ENGINES:
            for op in self.ops[e]:
                best = {}
                keep = []
                for d in op.deps:
                    if d.is_dma:
                        keep.append(d)
                        continue
                    b = best.get(d.eng)
                    if b is None or d.idx > b.idx:
                        best[d.eng] = d
                for d in best.values():
                    d.signal = True
                    keep.append(d)
                op.deps = keep
        for e in ENGINES:
            for op in self.ops[e]:
                if not op.is_dma and op.signal:
                    self.ecount[e] += 1
                    op.count = self.ecount[e]
        finals = list(self.dma_last.values())
        ops = self.ops
        esem, dsem = self.esem, self.dsem

        def target(d):
            if d.is_dma:
                return dsem[d.dsem], d.dval
            return esem[d.eng], d.count

        def body(ename):
            def run(eng):
                waited = {}
                for op in ops[ename]:
                    need = {}
                    for d in op.deps:
                        s, v = target(d)
                        if need.get(id(s), (None, 0))[1] < v:
                            need[id(s)] = (s, v)
                    for sid, (s, v) in need.items():
                        if waited.get(sid, 0) >= v:
                            continue
                        eng.wait_ge(s, v)
                        waited[sid] = v
                    ins = op.fn(eng)
                    if op.is_dma:
                        ins.then_inc(dsem[op.dsem], 16)
                    elif op.signal:
                        ins.then_inc(esem[ename], 1)
                for d in finals:
                    if d.eng != ename:
                        continue
                    s, v = target(d)
                    if waited.get(id(s), 0) >= v:
                        continue
                    eng.wait_ge(s, v)
                    waited[id(s)] = v

            return run

        with nc.Block() as block:
            for e in ENGINES:
                if not ops[e]:
                    continue
                getattr(block, e)(body(e))
        self._reset()


class Ring:
    def __init__(self, stack, nc, name, n, shape, dt):
        self.t = [stack.enter_context(nc.sbuf_tensor("%s%d" % (name, i), list(shape), dt)) for i in range(n)]
        self.keys = ["%s%d" % (name, i) for i in range(n)]
        self.i = 0

    def next(self):
        k = self.i % len(self.t)
        self.i += 1
        return self.t[k], self.keys[k]


class KB:
    def __init__(self, nc, S, PS):
        self.nc, self.S, self.PS = nc, S, PS
        self.psi = 0
        self.reserved = set()

    def bank(self, b):
        return self.PS[b // 2][:, (b % 2) * 512:(b % 2 + 1) * 512], "ps%d" % b

    def psnext(self):
        b = self.psi % 8
        self.psi += 1
        while b in self.reserved:
            b = self.psi % 8
            self.psi += 1
        return self.bank(b)

    def pspair(self):
        if self.psi % 2:
            self.psi += 1
        p = (self.psi % 8) // 2
        self.psi += 2
        return self.PS[p], ["ps%d" % (2 * p), "ps%d" % (2 * p + 1)]

    def dma(self, q, out, in_, r=(), w=()):
        return self.S.add(q, lambda e: e.dma_start(out=out, in_=in_), r=r, w=w, dma=True)

    def mm(self, out, lhsT, rhs, start, stop, r=(), w=()):
        return self.S.add("tensor", lambda e: e.matmul(out, lhsT, rhs, start=start, stop=stop), r=r, w=w)

    def tr(self, out, in_, ident, r=(), w=()):
        return self.S.add("tensor", lambda e: e.transpose(out, in_, ident), r=r, w=w)

    def act(self, out, in_, func, r=(), w=(), bias=None, scale=None, accum_out=None):
        kw = {}
        if bias is not None:
            kw["bias"] = bias
        if scale is not None:
            kw["scale"] = scale
        if accum_out is not None:
            kw["accum_out"] = accum_out
        return self.S.add("scalar", lambda e: e.activation(out=out, in_=in_, func=func, **kw), r=r, w=w)

    def tt(self, out, in0, in1, op, r=(), w=(), eng="vector"):
        return self.S.add(eng, lambda e: e.tensor_tensor(out=out, in0=in0, in1=in1, op=op), r=r, w=w)

    def ts(self, out, in0, s1, op0, r=(), w=(), s2=None, op1=None, eng="vector"):
        if op1 is None:
            return self.S.add(eng, lambda e: e.tensor_scalar(out=out, in0=in0, scalar1=s1, scalar2=None, op0=op0), r=r, w=w)
        return self.S.add(eng, lambda e: e.tensor_scalar(out=out, in0=in0, scalar1=s1, scalar2=s2, op0=op0, op1=op1), r=r, w=w)

    def stt(self, out, in0, scalar, in1, op0, op1, r=(), w=(), accum_out=None):
        if accum_out is None:
            return self.S.add("vector", lambda e: e.scalar_tensor_tensor(out=out, in0=in0, scalar=scalar, in1=in1, op0=op0, op1=op1), r=r, w=w)
        return self.S.add("vector", lambda e: e.scalar_tensor_tensor(out=out, in0=in0, scalar=scalar, in1=in1, op0=op0, op1=op1, accum_out=accum_out), r=r, w=w)

    def copy(self, out, in_, r=(), w=(), eng="vector"):
        if eng == "scalar":
            return self.S.add("scalar", lambda e: e.copy(out=out, in_=in_), r=r, w=w)
        return self.S.add(eng, lambda e: e.tensor_copy(out=out, in_=in_), r=r, w=w)

    def recip(self, out, in_, r=(), w=()):
        return self.S.add("vector", lambda e: e.reciprocal(out=out, in_=in_), r=r, w=w)

    def memset(self, out, val, r=(), w=(), eng="vector"):
        return self.S.add(eng, lambda e: e.memset(out, val), r=r, w=w)

    def reduce(self, out, in_, op, r=(), w=()):
        return self.S.add("vector", lambda e: e.tensor_reduce(out=out, in_=in_, axis=AX.X, op=op), r=r, w=w)


def build(upto=99, debug=False):
    nc = bass.Bass("TRN2", target_bir_lowering=False)

    def din(name, shape, dt=F32):
        return nc.dram_tensor(name, list(shape), dt, kind="ExternalInput").ap()

    def dscr(name, shape, dt):
        return nc.dram_tensor(name, list(shape), dt, kind="ExternalOutput" if debug else "Internal").ap()

    x = din("x", [T, D])
    ctx = din("ctx", [TC, D])
    cvec = din("cvec", [128, 16])
    w_ada_l = din("w_ada_l", [12, 128, 8 * 512])
    bada_b = din("bada_b", [128, 6 * D])
    gvecs_b = din("gvecs_b", [128, 3 * D])
    pvecs = din("pvecs", [128, 32 + 8 * 31])
    lamv_b = din("lamv_b", [128, 256])
    gsub_b = din("gsub_b", [128, 128])
    constf = din("constf", [128, 801])
    ropeC = din("ropeC", [128, T])
    ropeS = din("ropeS", [128, T])
    w_fm = din("w_fm", [72, 128, 1024])
    w3 = din("w3", [3, 128, 8 * 1024])
    w_router_l = din("w_router_l", [128, 8 * 16])
    if upto >= 6:
        wg_l = din("wg_l", [NE, 11, 128, 2048])
        wu_l = din("wu_l", [NE, 11, 128, 2048])
        wd_l = din("wd_l", [NE, 2, 128, NFB * 512])
    out = nc.dram_tensor("out", [T, D], F32, kind="ExternalOutput").ap()

    y_d = dscr("y_d", [8, 128, T], F32)
    sgc_d = dscr("sgc_d", [8, 128, T], BF16)
    sga_d = dscr("sga_d", [8, 128, T], BF16)
    o_d = dscr("o_d", [T, D], BF16)
    x2_d = [dscr("x2a_d", [T, 512], F32), dscr("x2b_d", [T, 512], F32)]
    hf_d = dscr("hf_d", [T, D], BF16)
    dbg = {}
    if debug:
        dbg["modL"] = dscr("dbg_modL", [128, 6 * D], F32)
        dbg["hT"] = dscr("dbg_hT", [128, 8 * TK], BF16)
        dbg["sso"] = dscr("dbg_sso", [128, 256], F32)
        dbg["lg"] = dscr("dbg_lg", [128, 512], F32)
        dbg["aff"] = dscr("dbg_aff", [128, 512], F32)
        dbg["pos"] = dscr("dbg_pos", [128, 512], F32)
        dbg["idx"] = dscr("dbg_idx", [128, 64], I32)
        dbg["gate"] = dscr("dbg_gate", [128, 64], F32)
        dbg["xs0"] = dscr("dbg_xs0", [128, 4 * D], BF16)
        dbg["hid0"] = dscr("dbg_hid0", [128, NFB * 512], BF16)
        dbg["ysg0"] = dscr("dbg_ysg0", [128, 512], F32)

    with contextlib.ExitStack() as top:
        S = Sched(nc, top)
        PS = [top.enter_context(nc.psum_tensor("ps%d" % i, [128, 1024], F32)) for i in range(4)]
        K = KB(nc, S, PS)

        def sb(stack, name, shape, dt):
            return stack.enter_context(nc.sbuf_tensor(name, list(shape), dt))

        cf = sb(top, "cf", [128, 801], F32)
        ident_f = cf[:, 0:128]
        iota_row = cf[:, 128:640]
        pidx = cf[:, 768:769]
        tileidx = cf[:, 769:801]
        ident_bf = sb(top, "ident_bf", [128, 128], BF16)
        tri_bf = sb(top, "tri_bf", [128, 128], BF16)
        ones_bf = sb(top, "ones_bf", [128, 128], BF16)
        ones_f = sb(top, "ones_f", [128, 128], F32)
        eps_t = sb(top, "eps_t", [128, 1], F32)
        modL = sb(top, "modL", [128, 6 * D], F32)
        SH1, G1, GM = modL[:, 0:D], modL[:, D:2 * D], modL[:, 2 * D:3 * D]
        SH2, G2, GF = modL[:, 3 * D:4 * D], modL[:, 4 * D:5 * D], modL[:, 5 * D:6 * D]
        pv = sb(top, "pv", [128, 32 + 8 * 31], F32)
        neg_lam = sb(top, "neg_lam", [128, 1], F32)
        gsub08 = sb(top, "gsub08", [128, 128], F32)
        ss_o = sb(top, "ss_o", [128, 256], F32)
        lg = sb(top, "lg", [128, 32, 16], F32)
        idx_all = sb(top, "idx_all", [128, 64], I32)
        gate_all = sb(top, "gate_all", [128, 64], F32)

        mid = contextlib.ExitStack()
        hT = sb(mid, "hT", [128, 8, TK], BF16)
        midc = contextlib.ExitStack()
        modC = sb(midc, "modC", [128, 2 * D], F32)

        with contextlib.ExitStack() as st:
            K.dma("sync", cf[:], constf, w=["cf"])
            K.dma("sync", pv[:], pvecs, w=["pv"])
            cv = sb(st, "cv", [128, 16], F32)
            K.dma("sync", cv[:], cvec, w=["cv"])
            bada = sb(st, "bada", [128, 6 * D], F32)
            K.dma("sync", bada[:], bada_b, w=["bada"])
            gv = sb(st, "gv", [128, 2 * D], F32)
            K.dma("sync", gv[:], gvecs_b[:, 0:2 * D], w=["gv"])
            lv = sb(st, "lv", [128, 256], F32)
            K.dma("sync", lv[:], lamv_b, w=["lv"])
            gs = sb(st, "gs", [128, 128], F32)
            K.dma("sync", gs[:], gsub_b, w=["gs"])

            K.copy(ident_bf[:], ident_f, r=["cf"], w=["ident_bf"])
            K.copy(tri_bf[:], cf[:, 640:768], r=["cf"], w=["tri_bf"])
            K.memset(ones_bf[:], 1.0, w=["ones_bf"])
            K.memset(ones_f[:], 1.0, w=["ones_f"])
            K.memset(eps_t[:], EPS, w=["eps_t"])
            K.memset(ss_o[:], 0.0, w=["ss_o"])
            K.ts(gsub08[:], gs[:], 1.0 - LAM_INIT, ALU.mult, r=["gs"], w=["gsub08"])

            pr = sb(st, "pr", [128, 128], F32)
            K.tt(pr[:, 0:64], lv[:, 0:64], lv[:, 64:128], ALU.mult, r=["lv"], w=["pr"])
            K.tt(pr[:, 64:128], lv[:, 128:192], lv[:, 192:256], ALU.mult, r=["lv", "pr"], w=["pr"])
            sl = sb(st, "sl", [128, 2], F32)
            K.reduce(sl[:], pr[:].rearrange("p (a b) -> p a b", a=2), ALU.add, r=["pr"], w=["sl"])
            el = sb(st, "el", [128, 2], F32)
            K.act(el[:], sl[:], AF.Exp, r=["sl"], w=["el"])
            dl = sb(st, "dl", [128, 1], F32)
            K.tt(dl[:], el[:, 1:2], el[:, 0:1], ALU.subtract, r=["el"], w=["dl"])
            K.ts(neg_lam[:], dl[:], -LAM_INIT, ALU.add, r=["dl"], w=["neg_lam"])

            sc = sb(st, "sc", [128, 16], F32)
            K.act(sc[:], cv[:], AF.Silu, r=["cv"], w=["sc"])
            rep = sb(st, "rep", [128, 16, 128], BF16)
            for kc in range(16):
                K.ts(rep[:, kc, :], ones_f[:], sc[:, kc:kc + 1], ALU.mult, r=["ones_f", "sc"], w=["rep"])
            wring = Ring(st, nc, "wada", 2, [128, 8, 512], BF16)
            for g in range(12):
                wb, wk = wring.next()
                K.dma("gpsimd", wb[:], w_ada_l[g].rearrange("p (k f) -> p k f", k=8), w=[wk])
                ps, pk = K.psnext()
                for kc in range(8):
                    K.mm(ps, rep[:, kc, :], wb[:, kc, :], kc == 0, kc == 7, r=["rep", wk], w=[pk])
                K.tt(modL[:, g * 512:(g + 1) * 512], ps, bada[:, g * 512:(g + 1) * 512], ALU.add,
                     r=[pk, "bada"], w=["modL%d" % g])
                if g < 4:
                    ps2, pk2 = K.psnext()
                    for kc in range(8):
                        K.mm(ps2, rep[:, 8 + kc, :], wb[:, kc, :], kc == 0, kc == 7, r=["rep", wk], w=[pk2])
                    K.tt(modC[:, g * 512:(g + 1) * 512], ps2, bada[:, g * 512:(g + 1) * 512], ALU.add,
                         r=[pk2, "bada"], w=["modC%d" % g])
            allL = ["modL%d" % g for g in range(12)]
            allC = ["modC%d" % g for g in range(4)]
            K.ts(G1, G1, 1.0, ALU.add, r=allL, w=allL)
            K.tt(G1, G1, gv[:, 0:D], ALU.mult, r=allL + ["gv"], w=allL)
            K.ts(G2, G2, 1.0, ALU.add, r=allL, w=allL)
            K.tt(G2, G2, gv[:, D:2 * D], ALU.mult, r=allL + ["gv"], w=allL)
            K.ts(modC[:, D:2 * D], modC[:, D:2 * D], 1.0, ALU.add, r=allC, w=allC)
            K.tt(modC[:, D:2 * D], modC[:, D:2 * D], gv[:, 0:D], ALU.mult, r=allC + ["gv"], w=allC)
            if debug:
                K.dma("sync", dbg["modL"], modL[:], r=allL)
            S.emit()
        if upto < 1:
            return nc

        with contextlib.ExitStack() as st:
            xr = Ring(st, nc, "xt", 3, [128, D], F32)
            t1r = Ring(st, nc, "t1_", 2, [128, D], F32)
            hbr = Ring(st, nc, "hb", 2, [128, D], BF16)
            jr = Ring(st, nc, "junk", 2, [128, D], BF16)
            ss = sb(st, "ss", [128, 3 * NKT], F32)
            K.memset(ss[:], 0.0, w=["ss%d" % i for i in range(NKT)])
            for i in range(NKT):
                src = x[i * 128:(i + 1) * 128, :] if i < NT else ctx[(i - NT) * 128:(i - NT + 1) * 128, :]
                xt, xk = xr.next()
                K.dma("sync", xt[:], src, w=[xk])
                jk_t, jk = jr.next()
                sk = "ss%d" % i
                K.act(jk_t[:], xt[:], AF.Square, r=[xk], w=[jk, sk], accum_out=ss[:, i:i + 1])
                K.act(ss[:, NKT + i:NKT + i + 1], ss[:, i:i + 1], AF.Sqrt, r=[sk], w=[sk], bias=eps_t[:], scale=1.0 / D)
                K.recip(ss[:, 2 * NKT + i:2 * NKT + i + 1], ss[:, NKT + i:NKT + i + 1], r=[sk], w=[sk])
                Gm, SHm = (G1, SH1) if i < NT else (modC[:, D:2 * D], modC[:, 0:D])
                t1, tk = t1r.next()
                K.stt(t1[:], xt[:], ss[:, 2 * NKT + i:2 * NKT + i + 1], Gm, ALU.mult, ALU.mult, r=[xk, sk], w=[tk])
                hb, hk = hbr.next()
                K.tt(hb[:], t1[:], SHm, ALU.add, r=[tk], w=[hk])
                ps, pk = K.psnext()
                tp = ps.bitcast(BF16).rearrange("p (a b) -> p a b", a=8)
                for kc in range(8):
                    K.tr(tp[:, kc, :], hb[:, kc * 128:(kc + 1) * 128], ident_bf[:], r=[hk], w=[pk])
                K.copy(hT[:, :, i * 128:(i + 1) * 128], tp, r=[pk], w=["hT%d" % i], eng="scalar" if i % 2 else "vector")
            if debug:
                K.dma("sync", dbg["hT"], hT[:].rearrange("p a b -> p (a b)"), r=["hT%d" % i for i in range(NKT)])
            S.emit()
        midc.close()
        if upto < 2:
            mid.close()
            return nc

        with contextlib.ExitStack() as st:
            wr_ = Ring(st, nc, "wblk", 2, [128, 4, 1024], BF16)
            ur = Ring(st, nc, "u", 2, [128, T + 30], BF16)
            dgr = Ring(st, nc, "dg", 2, [128, 31, 128], BF16)
            sgr = Ring(st, nc, "sg", 2, [128, 512], F32)
            gcr = Ring(st, nc, "gcc", 3, [128, 512], BF16)
            gar = Ring(st, nc, "gac", 3, [128, 512], BF16)
            ycr = Ring(st, nc, "yc", 3, [128, 512], F32)
            for ut, uk in zip(ur.t, ur.keys):
                K.memset(ut[:, 0:15], 0.0, w=[uk])
                K.memset(ut[:, T + 15:T + 30], 0.0, w=[uk])

            def conv(j, ub, uk, dgb, dk):
                for n in range(8):
                    pc, pk = K.psnext()
                    for k in range(31):
                        K.mm(pc, dgb[:, k, :], ub[:, n * 512 + k:n * 512 + k + 512], k == 0, k == 30, r=[dk, uk], w=[pk])
                    yc, yk = ycr.next()
                    K.act(yc[:], pc, AF.Identity, r=[pk], w=[yk], bias=pv[:, j:j + 1])
                    K.dma("sync", y_d[j, :, n * 512:(n + 1) * 512], yc[:], r=[yk])

            pend = None
            for j in range(8):
                wb, wk = wr_.next()
                K.dma("gpsimd", wb[:], w_fm[j * 4:(j + 1) * 4].rearrange("b p f -> p b f"), w=[wk])
                ub, uk = ur.next()
                for n in range(8):
                    cs = slice(n * 512, (n + 1) * 512)
                    pss = []
                    for b in range(4):
                        ps, pk = K.psnext()
                        for kc in range(8):
                            K.mm(ps, wb[:, b, kc * 128:(kc + 1) * 128], hT[:, kc, cs], kc == 0, kc == 7, r=[wk], w=[pk])
                        pss.append((ps, pk))
                    sg, sk = sgr.next()
                    K.act(sg[:], pss[1][0], AF.Sigmoid, r=[pss[1][1]], w=[sk])
                    K.tt(ub[:, 15 + n * 512:15 + (n + 1) * 512], pss[0][0], sg[:], ALU.mult, r=[pss[0][1], sk], w=[uk])
                    gc, gk = gcr.next()
                    K.act(gc[:], pss[2][0], AF.Sigmoid, r=[pss[2][1]], w=[gk])
                    K.dma("sync", sgc_d[j, :, cs], gc[:], r=[gk])
                    ga, gak = gar.next()
                    K.act(ga[:], pss[3][0], AF.Sigmoid, r=[pss[3][1]], w=[gak])
                    K.dma("sync", sga_d[j, :, cs], ga[:], r=[gak])
                dgb, dk = dgr.next()
                for k in range(31):
                    K.ts(dgb[:, k, :], ident_bf[:], pv[:, 32 + j * 31 + k:32 + j * 31 + k + 1], ALU.mult, w=[dk])
                if pend is not None:
                    conv(*pend)
                pend = (j, ub, uk, dgb, dk)
            conv(*pend)
            S.emit()
        if upto < 3:
            mid.close()
            return nc

        with contextlib.ExitStack() as st:
            rC = sb(st, "rC", [128, T], F32)
            rS = sb(st, "rS", [128, T], F32)
            K.dma("sync", rC[:], ropeC, w=["rC"])
            K.dma("sync", rS[:], ropeS, w=["rS"])
            hwr = Ring(st, nc, "hw", 1, [128, 5, 1024], BF16)
            qTr = Ring(st, nc, "qT", 1, [128, 2, T], BF16)
            kTr = Ring(st, nc, "kT", 1, [128, TK], BF16)
            Var = Ring(st, nc, "Va", 1, [128, NKT, 129], BF16)
            ostr = Ring(st, nc, "ost", 1, [128, NT, 128], BF16)
            t1r = Ring(st, nc, "ra", 2, [128, 512], F32)
            t2r = Ring(st, nc, "rb", 2, [128, 512], F32)
            PTr = Ring(st, nc, "PT", 3, [128, 8, 128], BF16)
            o1r = Ring(st, nc, "o1", 2, [128, 128], F32)
            odr = Ring(st, nc, "od", 2, [128, 128], F32)
            jr = Ring(st, nc, "jk", 2, [128, 128], F32)
            rvr = Ring(st, nc, "rv", 4, [128, 2], F32)
            for vt, vk in zip(Var.t, Var.keys):
                K.memset(vt[:, :, 128:129], 1.0, w=[vk])
            qbr = Ring(st, nc, "qb", 4, [128, 512], BF16)
            Pm = sb(st, "Pm", [128, 128], BF16)
            Pm4 = Pm[:].rearrange("p (m h c) -> p m h c", h=2, c=16)
            id4 = ident_bf[:].rearrange("p (m h c) -> p m h c", h=2, c=16)
            K.copy(Pm4[:, :, 0, :], id4[:, :, 1, :], w=["Pm"])
            K.copy(Pm4[:, :, 1, :], id4[:, :, 0, :], r=["Pm"], w=["Pm"])
            for qt_, qk_ in zip(qTr.t, qTr.keys):
                K.memset(qt_[64:128, 0, :], 0.0, w=[qk_])
                K.memset(qt_[0:64, 1, :], 0.0, w=[qk_])
            groups = [(k0, min(k0 + 8, NKT)) for k0 in range(0, NKT, 8)]
            for h in range(8):
                hw, hwk = hwr.next()
                K.dma("gpsimd", hw[:], w_fm[32 + h * 5:32 + (h + 1) * 5].rearrange("b p f -> p b f"), w=[hwk])
                qT, qk = qTr.next()
                kT, kk = kTr.next()
                Va, vk = Var.next()
                ost, ok = ostr.next()

                def proj(blk, cs, n):
                    ps, pk = K.psnext()
                    for kc in range(8):
                        K.mm(ps[:, 0:n], hw[:, blk, kc * 128:(kc + 1) * 128], hT[:, kc, cs], kc == 0, kc == 7, r=[hwk], w=[pk])
                    return ps, pk

                def finish_rope(item):
                    cs, lst = item
                    for (b0, dst, dk, p0, k0_, qb, qbk) in lst:
                        p1, k1_ = K.psnext()
                        K.mm(p1, Pm[:], qb[:], True, True, r=[qbk, "Pm"], w=[k1_])
                        ta, tak = t1r.next()
                        tb, tbk = t2r.next()
                        K.tt(ta[:], p0, rC[:, cs], ALU.mult, r=[k0_, "rC", qbk], w=[tak])
                        K.tt(tb[:], p1, rS[:, cs], ALU.mult, r=[k1_, "rS"], w=[tbk])
                        if b0 == 0:
                            K.tt(dst[0:64, 0, cs], ta[0:64, :], tb[0:64, :], ALU.add, r=[tak, tbk], w=[dk])
                            K.tt(dst[64:128, 1, cs], ta[64:128, :], tb[64:128, :], ALU.add, r=[tak, tbk], w=[dk])
                        else:
                            K.tt(dst[:, cs], ta[:], tb[:], ALU.add, r=[tak, tbk], w=[dk])

                pend_r = None
                for n in range(8):
                    cs = slice(n * 512, (n + 1) * 512)
                    lst = []
                    for (b0, dst, dk) in ((0, qT, qk), (2, kT, kk)):
                        p0, k0_ = proj(b0, cs, 512)
                        qb, qbk = qbr.next()
                        K.copy(qb[:], p0, r=[k0_], w=[qbk], eng="scalar")
                        lst.append((b0, dst, dk, p0, k0_, qb, qbk))
                    if pend_r is not None:
                        finish_rope(pend_r)
                    pend_r = (cs, lst)
                finish_rope(pend_r)
                pc_, pck = proj(2, slice(T, TK), 256)
                K.copy(kT[:, T:TK], pc_[:, 0:256], r=[pck], w=[kk])
                for g0 in range(0, NKT, 4):
                    g1 = min(g0 + 4, NKT)
                    ps, pk = K.psnext()
                    for ti in range(g0, g1):
                        for kc in range(8):
                            K.mm(ps[:, (ti - g0) * 128:(ti - g0 + 1) * 128], hT[:, kc, ti * 128:(ti + 1) * 128],
                                 hw[:, 4, kc * 128:(kc + 1) * 128], kc == 0, kc == 7, r=[hwk], w=[pk])
                    K.copy(Va[:, g0:g1, 0:128], ps[:, 0:(g1 - g0) * 128].rearrange("p (a b) -> p a b", b=128), r=[pk], w=[vk])

                units = [(qt, sub, gi, k0, k1) for qt in range(NT) for sub in range(2) for gi, (k0, k1) in enumerate(groups)]
                o1s = {}
                pts = {}

                def scbuf(u):
                    return PS[u % 3], ["ps%d" % (2 * (u % 3)), "ps%d" % (2 * (u % 3) + 1)]

                def emitS(u):
                    qt, sub, gi, k0, k1 = units[u]
                    prt = slice(sub * 64, (sub + 1) * 64)
                    qs = slice(qt * 128, (qt + 1) * 128)
                    sc_, sck = scbuf(u)
                    for kt in range(k0, k1):
                        K.mm(sc_[:, (kt - k0) * 128:(kt - k0 + 1) * 128], kT[:, kt * 128:(kt + 1) * 128], qT[:, sub, qs],
                             True, True, r=[kk, qk], w=sck)

                def emitExp(u):
                    qt, sub, gi, k0, k1 = units[u]
                    sc_, sck = scbuf(u)
                    pt, ptk = PTr.next()
                    ng = k1 - k0
                    K.act(pt[:, 0:ng, :].rearrange("p a b -> p (a b)"), sc_[:, 0:ng * 128], AF.Exp, r=sck, w=[ptk], scale=0.125)
                    pts[u] = (pt, ptk)

                def emitPV(u):
                    qt, sub, gi, k0, k1 = units[u]
                    pt, ptk = pts.pop(u)
                    ab = 6 + (qt * 2 + sub) % 2
                    acc, acck = K.bank(ab)
                    acc = acc[:, 0:129]
                    for kt in range(k0, k1):
                        K.mm(acc, pt[:, kt - k0, :], Va[:, kt, :], kt == 0, kt == NKT - 1, r=[ptk, vk], w=[acck])
                    if gi != len(groups) - 1:
                        return
                    if sub == 0:
                        o1s[qt] = o1r.next()
                    o1, o1k = o1s[qt]
                    rv, rvk = rvr.next()
                    K.recip(rv[:, 0:1], acc[:, 128:129], r=[acck], w=[rvk])
                    if sub == 0:
                        K.ts(o1[:], acc[:, 0:128], rv[:, 0:1], ALU.mult, r=[acck, rvk], w=[o1k])
                    else:
                        K.tt(rv[:, 0:1], rv[:, 0:1], neg_lam[:], ALU.mult, r=[rvk], w=[rvk])
                        od, odk = odr.next()
                        K.stt(od[:], acc[:, 0:128], rv[:, 0:1], o1[:], ALU.mult, ALU.add, r=[acck, rvk, o1k], w=[odk])
                        jt, jk = jr.next()
                        col = qt * 8 + h
                        K.stt(jt[:], od[:], 1.0, od[:], ALU.mult, ALU.mult, r=[odk], w=[jk, "sso%d" % col],
                              accum_out=ss_o[:, col:col + 1])
                        K.tt(ost[:, qt, :], od[:], gsub08[:], ALU.mult, r=[odk], w=[ok])

                emitS(0)
                emitS(1)
                for u in range(len(units)):
                    if u + 2 < len(units):
                        emitS(u + 2)
                    emitExp(u)
                    emitPV(u)
                K.dma("sync", o_d.rearrange("(t p) c -> p t c", p=128)[:, :, h * 128:(h + 1) * 128], ost[:], r=[ok])
            if debug:
                K.dma("sync", dbg["sso"], ss_o[:], r=["sso%d" % c for c in range(256)])
            S.emit()
        mid.close()
        if upto < 4:
            return nc

        with contextlib.ExitStack() as st:
            wco = sb(st, "wco", [128, 8, 1024], BF16)
            wao = sb(st, "wao", [128, 8, 1024], BF16)
            wout = sb(st, "wout", [128, 8, 1024], BF16)
            for i_, (wt, nm) in enumerate(((wco, "wco"), (wao, "wao"), (wout, "wout"))):
                K.dma("gpsimd", wt[:], w3[i_].rearrange("p (k f) -> p k f", k=8), w=[nm])
            wrt = sb(st, "wrt", [128, 8, 16], F32)
            K.dma("sync", wrt[:], w_router_l.rearrange("p (k f) -> p k f", k=8), w=["wrt"])
            rstd_o = sb(st, "rstd_o", [128, 256], F32)
            K.act(rstd_o[:], ss_o[:], AF.Sqrt, w=["rstd_o"], bias=eps_t[:], scale=1.0 / 128)
            K.recip(rstd_o[:], rstd_o[:], r=["rstd_o"], w=["rstd_o"])
            CH = 256
            NCH = T // CH
            TPC = CH // 128
            yTr = Ring(st, nc, "yT", 2, [128, 8, CH], F32)
            ysqr = Ring(st, nc, "ysq", 1, [128, 8, CH], BF16)
            ybr = Ring(st, nc, "ybf", 1, [128, 8, CH], BF16)
            lnr = Ring(st, nc, "ln", 5, [128, CH], F32)
            tCa = sb(st, "tCa", [128, 8, CH], BF16)
            zTr = Ring(st, nc, "zT", 2, [128, 8, CH], BF16)
            sgcr = Ring(st, nc, "sgc", 2, [128, 8, CH], BF16)
            sgar = Ring(st, nc, "sga", 2, [128, 8, CH], BF16)
            m1r = Ring(st, nc, "m1", 2, [128, 8, CH], BF16)
            otr = Ring(st, nc, "ot", 1, [128, D], BF16)
            onb = sb(st, "onb", [128, 8, 128], BF16)
            oTr = Ring(st, nc, "oT", 2, [128, 8, CH], BF16)
            mixr = Ring(st, nc, "mixin", 2, [128, 8, CH], BF16)
            xr = Ring(st, nc, "x4", 1, [128, D], F32)
            x1r = Ring(st, nc, "x1_", 2, [128, D], F32)
            j4 = sb(st, "j4", [128, D], BF16)
            ssf = sb(st, "ssf", [128, 3 * NT], F32)
            K.memset(ssf[:], 0.0, w=["ssf%d" % i for i in range(NT)])
            hf32r = Ring(st, nc, "hf32", 2, [128, D], F32)
            hfbr = Ring(st, nc, "hfb", 1, [128, D], BF16)
            hfTr = Ring(st, nc, "hfT", 2, [128, 8, 128], F32)
            lgTr = Ring(st, nc, "lgT", 1, [16, 128], F32)
            stz, stm, sto, stx = {}, {}, {}, {}
            woutf, wfk = hf32r.t[0], hf32r.keys[0]
            for kc in range(8):
                K.copy(woutf[:], wout[:, kc, :], r=["wout", wfk], w=[wfk])
                K.tt(wout[:, kc, :], woutf[:], GM, ALU.mult, r=[wfk, "wout"], w=["wout"])

            pre_y, pre_c, pre_a = {}, {}, {}

            def load_y(n):
                t_, k_ = yTr.next()
                K.dma("gpsimd", t_[:], y_d[:, :, n * CH:(n + 1) * CH].rearrange("j p t -> p j t"), w=[k_])
                pre_y[n] = (t_, k_)

            def load_c(n):
                t_, k_ = sgcr.next()
                K.dma("gpsimd", t_[:], sgc_d[:, :, n * CH:(n + 1) * CH].rearrange("j p t -> p j t"), w=[k_])
                pre_c[n] = (t_, k_)

            def load_a(n):
                t_, k_ = sgar.next()
                K.dma("gpsimd", t_[:], sga_d[:, :, n * CH:(n + 1) * CH].rearrange("j p t -> p j t"), w=[k_])
                pre_a[n] = (t_, k_)

            load_y(0)
            load_c(0)
            load_a(0)

            def phaseA(n):
                cs = slice(n * CH, (n + 1) * CH)
                yT, yk = pre_y.pop(n)
                if n + 1 < NCH:
                    load_y(n + 1)
                p1, p1k = K.psnext()
                p2, p2k = K.psnext()
                yb, ybk = ybr.next()
                yq, yqk = ysqr.next()
                K.copy(yb[:], yT[:], r=[yk], w=[ybk])
                K.act(yq[:], yT[:], AF.Square, r=[yk], w=[yqk])
                for j in range(8):
                    K.mm(p1[:, 0:CH], ones_bf[:], yb[:, j, :], j == 0, j == 7, r=[ybk], w=[p1k])
                for j in range(8):
                    K.mm(p2[:, 0:CH], ones_bf[:], yq[:, j, :], j == 0, j == 7, r=[yqk], w=[p2k])
                mean, mk = lnr.next()
                K.ts(mean[:], p1[:, 0:CH], 1.0 / D, ALU.mult, r=[p1k], w=[mk])
                msq, msk = lnr.next()
                K.tt(msq[:], mean[:], mean[:], ALU.mult, r=[mk], w=[msk])
                var, vk_ = lnr.next()
                K.stt(var[:], p2[:, 0:CH], 1.0 / D, msq[:], ALU.mult, ALU.subtract, r=[p2k, msk], w=[vk_])
                sd, sdk = lnr.next()
                K.act(sd[:], var[:], AF.Sqrt, r=[vk_], w=[sdk], bias=eps_t[:], scale=1.0)
                rstd, rk = lnr.next()
                K.recip(rstd[:], sd[:], r=[sdk], w=[rk])
                zT, zk = zTr.next()
                bc = [128, 8, CH]
                K.tt(yT[:], yT[:], mean[:].unsqueeze(1).to_broadcast(bc), ALU.subtract, r=[yk, mk], w=[yk])
                K.tt(yT[:], yT[:], rstd[:].unsqueeze(1).to_broadcast(bc), ALU.mult, r=[yk, rk], w=[yk])
                for j in range(8):
                    K.act(zT[:, j, :], yT[:, j, :], AF.Silu, r=[yk], w=[zk], bias=pv[:, 16 + j:17 + j], scale=pv[:, 8 + j:9 + j])
                stz[n] = (zT, zk)

            def phaseB(n):
                cs = slice(n * CH, (n + 1) * CH)
                zT, zk = stz.pop(n)
                sgc, sgck = pre_c.pop(n)
                if n + 1 < NCH:
                    load_c(n + 1)
                m1, m1k = m1r.next()
                for jb in range(8):
                    pc, pk = K.psnext()
                    for kc in range(8):
                        K.mm(pc[:, 0:CH], wco[:, kc, jb * 128:(jb + 1) * 128], zT[:, kc, :], kc == 0, kc == 7, r=["wco", zk], w=[pk])
                    K.stt(m1[:, jb, :], pc[:, 0:CH], pv[:, 24 + jb:25 + jb], sgc[:, jb, :], ALU.add, ALU.mult,
                          r=[pk, sgck], w=[m1k])
                oT, oTk = oTr.next()
                for tl in range(TPC):
                    tile = n * TPC + tl
                    ot, otk = otr.next()
                    K.dma("gpsimd", ot[:], o_d[tile * 128:(tile + 1) * 128, :], w=[otk])
                    K.tt(onb[:], ot[:].rearrange("p (a b) -> p a b", a=8),
                         rstd_o[:, tile * 8:(tile + 1) * 8].unsqueeze(2).to_broadcast([128, 8, 128]), ALU.mult,
                         r=[otk, "rstd_o"], w=["onb"])
                    ps, pk = K.psnext()
                    tp = ps.bitcast(BF16).rearrange("p (a b) -> p a b", a=8)
                    for kc in range(8):
                        K.tr(tp[:, kc, :], onb[:, kc, :], ident_bf[:], r=["onb"], w=[pk])
                    K.copy(oT[:, :, tl * 128:(tl + 1) * 128], tp, r=[pk], w=[oTk], eng="scalar")
                stm[n] = (m1, m1k)
                sto[n] = (oT, oTk)

            def phaseC(n):
                cs = slice(n * CH, (n + 1) * CH)
                m1, m1k = stm.pop(n)
                oT, oTk = sto.pop(n)
                sga, sgak = pre_a.pop(n)
                if n + 1 < NCH:
                    load_a(n + 1)
                mixin, mxk = mixr.next()
                for jb in range(8):
                    pa, pk = K.psnext()
                    for kc in range(8):
                        K.mm(pa[:, 0:CH], wao[:, kc, jb * 128:(jb + 1) * 128], oT[:, kc, :], kc == 0, kc == 7, r=["wao", oTk], w=[pk])
                    K.tt(tCa[:, jb, :], pa[:, 0:CH], sga[:, jb, :], ALU.mult, r=[pk, sgak], w=["tCa"])
                K.tt(mixin[:], tCa[:], m1[:], ALU.add, r=["tCa", m1k], w=[mxk])
                stx[n] = (mixin, mxk)

            d3q, d4q = [], []

            def phaseD(n):
                mixin, mxk = stx.pop(n)
                for tl in range(TPC):
                    tile = n * TPC + tl
                    xt, xk = xr.next()
                    K.dma("gpsimd", xt[:], x[tile * 128:(tile + 1) * 128, :], w=[xk])
                    x1, x1k = x1r.next()
                    x1ks = [x1k + "_0", x1k + "_1"]
                    for cg in range(2):
                        cgs = slice(cg * 512, (cg + 1) * 512)
                        pm, pk = K.psnext()
                        for kc in range(8):
                            K.mm(pm, mixin[:, kc, tl * 128:(tl + 1) * 128], wout[:, kc, cgs], kc == 0, kc == 7, r=[mxk, "wout"], w=[pk])
                        K.tt(x1[:, cgs], pm, xt[:, cgs], ALU.add, r=[pk, xk], w=[x1ks[cg]])
                    for cg in range(2):
                        K.dma("sync", x2_d[cg][tile * 128:(tile + 1) * 128, :], x1[:, cg * 512:(cg + 1) * 512], r=[x1ks[cg]])
                    sk = "ssf%d" % tile
                    K.act(j4[:], x1[:], AF.Square, r=x1ks, w=["j4", sk], accum_out=ssf[:, tile:tile + 1])
                    K.act(ssf[:, NT + tile:NT + tile + 1], ssf[:, tile:tile + 1], AF.Sqrt, r=[sk], w=[sk], bias=eps_t[:], scale=1.0 / D)
                    K.recip(ssf[:, 2 * NT + tile:2 * NT + tile + 1], ssf[:, NT + tile:NT + tile + 1], r=[sk], w=[sk])
                    hf32, hfk = hf32r.next()
                    K.stt(hf32[:], x1[:], ssf[:, 2 * NT + tile:2 * NT + tile + 1], G2, ALU.mult, ALU.mult, r=x1ks + [sk], w=[hfk])
                    K.tt(hf32[:], hf32[:], SH2, ALU.add, r=[hfk], w=[hfk])
                    hfb, hbk = hfbr.next()
                    K.copy(hfb[:], hf32[:], r=[hfk], w=[hbk])
                    K.dma("sync", hf_d[tile * 128:(tile + 1) * 128, :], hfb[:], r=[hbk])
                    d3q.append((tile, hf32, hfk))
                    if len(d3q) > 1:
                        phaseD3(*d3q.pop(0))
                    if len(d4q) > 1:
                        phaseD4(*d4q.pop(0))

            def phaseD3(tile, hf32, hfk):
                pp, ppk = K.pspair()
                tpf = pp[:].rearrange("p (a b) -> p a b", a=8)
                for kc in range(8):
                    K.tr(tpf[:, kc, :], hf32[:, kc * 128:(kc + 1) * 128], ident_f, r=[hfk], w=ppk)
                hfT, hfTk = hfTr.next()
                K.copy(hfT[:], tpf, r=ppk, w=[hfTk], eng="scalar")
                d4q.append((tile, hfT, hfTk))

            def phaseD4(tile, hfT, hfTk):
                pl, plk = K.psnext()
                for kc in range(8):
                    K.mm(pl[0:16, 0:128], wrt[:, kc, :], hfT[:, kc, :], kc == 0, kc == 7, r=[hfTk, "wrt"], w=[plk])
                lgT, lgTk = lgTr.next()
                K.copy(lgT[:], pl[0:16, 0:128], r=[plk], w=[lgTk], eng="scalar")
                pl2, pl2k = K.psnext()
                K.tr(pl2[:, 0:16], lgT[:], ident_f[0:16, 0:16], r=[lgTk], w=[pl2k])
                K.copy(lg[:, tile, :], pl2[:, 0:16], r=[pl2k], w=["lg%d" % tile])

            for s_ in range(NCH + 3):
                if s_ < NCH:
                    phaseA(s_)
                if 0 <= s_ - 1 < NCH:
                    phaseB(s_ - 1)
                if 0 <= s_ - 2 < NCH:
                    phaseC(s_ - 2)
                if 0 <= s_ - 3 < NCH:
                    phaseD(s_ - 3)
            while d3q:
                phaseD3(*d3q.pop(0))
            while d4q:
                phaseD4(*d4q.pop(0))
            if debug:
                K.dma("sync", dbg["lg"], lg[:].rearrange("p a b -> p (a b)"), r=["lg%d" % t_ for t_ in range(NT)])
            S.emit()
        if upto < 5:
            return nc

        late = contextlib.ExitStack()
        pos = sb(late, "pos", [128, 32, 16], F32)
        vals = sb(late, "vals", [128, 32, 16, 5], BF16)
        selr = Ring(late, nc, "sel", 6, [128, 512], BF16)
        c5r = Ring(late, nc, "c5", 2, [128, 5], F32)
        idxf = sb(late, "idxf", [128, 64], F32)
        g1 = sb(late, "g1", [128, 64], F32)

        sels = {}

        def comp_sel(e, i0, i1):
            for i in range(i0, i1):
                sl_, slk = selr.next()
                K.ts(sl_[:], iota_row, pos[:, i, e:e + 1], ALU.is_equal, r=["pos"], w=[slk])
                sels[(e, i)] = (sl_, slk)

        def comp_steps(e, i0, i1, pa_, pak):
            pacc = [pa_[:, 128 * q_:128 * q_ + 5] for q_ in range(4)]
            for i in range(i0, i1):
                if (e, i) in sels:
                    sl_, slk = sels.pop((e, i))
                else:
                    sl_, slk = selr.next()
                    K.ts(sl_[:], iota_row, pos[:, i, e:e + 1], ALU.is_equal, r=["pos"], w=[slk])
                for s4 in range(4):
                    K.mm(pacc[s4], sl_[:, s4 * 128:(s4 + 1) * 128], vals[:, i, e, :], i == 0 and s4 == 0, i == 31 and s4 == 3,
                         r=[slk, "vals"], w=[pak])
            if i1 < 32:
                return
            for s4 in range(4):
                col = e * 4 + s4
                c5, c5k = c5r.next()
                K.copy(c5[:], pacc[s4], r=[pak], w=[c5k])
                K.stt(idxf[:, col:col + 1], c5[:, 1:2], 128.0, c5[:, 0:1], ALU.mult, ALU.add, r=[c5k], w=["idxf%d" % col])
                K.copy(idx_all[:, col:col + 1], idxf[:, col:col + 1], r=["idxf%d" % col], w=["idx%d" % col])
                K.tt(g1[:, col:col + 1], c5[:, 4:5], c5[:, 3:4], ALU.add, r=[c5k], w=["g1%d" % col])
                K.tt(gate_all[:, col:col + 1], g1[:, col:col + 1], c5[:, 2:3], ALU.add, r=[c5k, "g1%d" % col], w=["gate%d" % col])

        with contextlib.ExitStack() as st:
            def b16(ap):
                return ap.unsqueeze(1).to_broadcast([128, 32, 16])

            def b32(ap):
                return ap.unsqueeze(2).to_broadcast([128, 32, 16])

            mx = sb(st, "mx", [128, 32], F32)
            K.reduce(mx[:], lg[:], ALU.max, w=["mx"])
            ex = sb(st, "ex", [128, 32, 16], F32)
            K.tt(ex[:], lg[:], b32(mx[:]), ALU.subtract, r=["mx"], w=["ex"])
            K.act(ex[:], ex[:], AF.Exp, r=["ex"], w=["ex"])
            sm = sb(st, "sm", [128, 32], F32)
            K.reduce(sm[:], ex[:], ALU.add, r=["ex"], w=["sm"])
            K.recip(sm[:], sm[:], r=["sm"], w=["sm"])
            aff = sb(st, "aff", [128, 32, 16], F32)
            K.tt(aff[:], ex[:], b32(sm[:]), ALU.mult, r=["ex", "sm"], w=["aff"])
            lo = sb(st, "lo", [128, 16], F32)
            K.memset(lo[:], 0.0, w=["lo"])
            cand = sb(st, "cand", [128, 16], F32)
            cmpr = Ring(st, nc, "cmp", 2, [128, 32, 16], BF16)
            cnt = sb(st, "cnt", [128, 16], F32)
            sel_ = sb(st, "sel_", [128, 16], F32)
            for kb in range(1, NBIS + 1):
                s_ = 2.0 ** (-kb)
                K.ts(cand[:], lo[:], s_, ALU.add, r=["lo"], w=["cand"])
                cmp_, ck = cmpr.next()
                K.tt(cmp_[:], aff[:], b16(cand[:]), ALU.is_gt, r=["aff", "cand"], w=[ck])
                pcn, pk = K.psnext()
                K.mm(pcn, ones_bf[:], cmp_[:].rearrange("p a b -> p (a b)"), True, True, r=[ck], w=[pk])
                K.reduce(cnt[:], pcn.rearrange("p (t e) -> p e t", e=16), ALU.add, r=[pk], w=["cnt"])
                K.ts(sel_[:], cnt[:], float(CAP) - 0.5, ALU.is_gt, r=["cnt"], w=["sel_"], s2=s_, op1=ALU.mult)
                K.tt(lo[:], lo[:], sel_[:], ALU.add, r=["lo", "sel_"], w=["lo"])
            maskf = sb(st, "maskf", [128, 32, 16], F32)
            K.tt(maskf[:], aff[:], b16(lo[:]), ALU.is_gt, r=["aff", "lo"], w=["maskf"])
            maskb = sb(st, "maskb", [128, 32, 16], BF16)
            K.copy(maskb[:], maskf[:], r=["maskf"], w=["maskb"])
            ppre, ppk = K.psnext()
            K.mm(ppre, tri_bf[:], maskb[:].rearrange("p a b -> p (a b)"), True, True, r=["maskb"], w=[ppk])
            ptot, ptk = K.psnext()
            K.mm(ptot, ones_bf[:], maskb[:].rearrange("p a b -> p (a b)"), True, True, r=["maskb"], w=[ptk])
            tot = sb(st, "tot", [128, 32, 16], F32)
            K.copy(tot[:].rearrange("p a b -> p (a b)"), ptot, r=[ptk], w=["tot"])
            offs = sb(st, "offs", [128, 32, 16], F32)
            K.memset(offs[:, 0, :], 0.0, w=["offs"])
            for i in range(1, 32):
                K.tt(offs[:, i, :], offs[:, i - 1, :], tot[:, i - 1, :], ALU.add, r=["offs", "tot"], w=["offs"])
            K.tt(pos[:].rearrange("p a b -> p (a b)"), ppre, offs[:].rearrange("p a b -> p (a b)"), ALU.add, r=[ppk, "offs"], w=["pos"])
            K.tt(pos[:], pos[:], maskf[:], ALU.mult, r=["pos", "maskf"], w=["pos"])
            K.ts(pos[:], pos[:], -1.0, ALU.add, r=["pos"], w=["pos"])
            K.copy(vals[:, :, :, 0], pidx.unsqueeze(2).to_broadcast([128, 32, 16]), w=["vals"])
            K.copy(vals[:, :, :, 1], b32(tileidx), r=["vals"], w=["vals"])
            K.copy(vals[:, :, :, 2], aff[:], r=["aff", "vals"], w=["vals"])
            h32 = sb(st, "h32", [128, 32, 16], F32)
            r1 = sb(st, "r1", [128, 32, 16], F32)
            K.copy(h32[:], vals[:, :, :, 2], r=["vals"], w=["h32"])
            K.tt(r1[:], aff[:], h32[:], ALU.subtract, r=["aff", "h32"], w=["r1"])
            K.copy(vals[:, :, :, 3], r1[:], r=["r1", "vals"], w=["vals"])
            K.copy(h32[:], vals[:, :, :, 3], r=["vals", "h32"], w=["h32"])
            K.tt(r1[:], r1[:], h32[:], ALU.subtract, r=["r1", "h32"], w=["r1"])
            K.copy(vals[:, :, :, 4], r1[:], r=["r1", "vals"], w=["vals"])
            NPRE = NE if debug and upto < 6 else 2
            for e in range(NPRE):
                pa_, pak = K.psnext()
                comp_steps(e, 0, 32, pa_, pak)
            if debug:
                K.dma("sync", dbg["aff"], aff[:].rearrange("p a b -> p (a b)"), r=["aff"])
                K.dma("sync", dbg["pos"], pos[:].rearrange("p a b -> p (a b)"), r=["pos"])
                K.dma("sync", dbg["idx"], idx_all[:], r=["idx%d" % c for c in range(64)])
                K.dma("sync", dbg["gate"], gate_all[:], r=["gate%d" % c for c in range(64)])
            S.emit()
        if upto < 6:
            late.close()
            return nc

        with contextlib.ExitStack() as st:
            xsr = Ring(st, nc, "xs", 2, [128, 4, D], BF16)
            xsTr = Ring(st, nc, "xsT", 2, [128, 8, 512], BF16)
            wgr = Ring(st, nc, "wg", 4, [128, 8, 256], BF16)
            wur = Ring(st, nc, "wu", 4, [128, 8, 256], BF16)
            wdr = Ring(st, nc, "wd", 2, [128, NFB, 512], BF16)
            hidr = Ring(st, nc, "hid", 2, [128, NFB, 512], BF16)
            sgtr = Ring(st, nc, "sgt", 2, [128, 512], F32)
            ysgr = Ring(st, nc, "ysg", 2, [128, 512], F32)
            loads = []
            for e in range(NE):
                for c in range(11):
                    loads.append(("g", e, c))
                    loads.append(("u", e, c))
                    if c == 4:
                        loads.append(("d", e, 0))
                    if c == 9:
                        loads.append(("d", e, 1))
            issued = {}
            state = {"i": 0}

            def issue_until(key):
                while key not in issued:
                    kind, e, c = loads[state["i"]]
                    state["i"] += 1
                    if kind == "g":
                        t_, k_ = wgr.next()
                        K.dma("gpsimd", t_[:], wg_l[e, c].rearrange("p (k f) -> p k f", k=8), w=[k_])
                    elif kind == "u":
                        t_, k_ = wur.next()
                        K.dma("gpsimd", t_[:], wu_l[e, c].rearrange("p (k f) -> p k f", k=8), w=[k_])
                    else:
                        t_, k_ = wdr.next()
                        K.dma("gpsimd", t_[:], wd_l[e, c].rearrange("p (k f) -> p k f", k=NFB), w=[k_])
                    issued[(kind, e, c)] = (t_, k_)

            pos_of = {tuple(k_): i_ for i_, k_ in enumerate(loads)}

            def issue_to(idx):
                tgt = min(idx + 1, len(loads))
                while state["i"] < tgt:
                    issue_until(tuple(loads[state["i"]]))

            gathered = {}

            def gather(e):
                xs_, xk_ = xsr.next()
                for s4 in range(4):
                    col = e * 4 + s4
                    S.add("gpsimd", (lambda xs_=xs_, s4=s4, col=col: lambda g: g.indirect_dma_start(
                        out=xs_[:, s4, :], out_offset=None, in_=hf_d,
                        in_offset=bass.IndirectOffsetOnAxis(ap=idx_all[:, col:col + 1], axis=0)))(),
                        r=["idx%d" % col], w=[xk_ + "_%d" % s4], dma=True)
                gathered[e] = (xs_, xk_)

            xsTs = {}

            def transp(e):
                xs_, xk_ = gathered.pop(e)
                xsT, xTk = xsTr.next()
                if debug and e == 0:
                    K.dma("sync", dbg["xs0"], xs_[:].rearrange("p a b -> p (a b)"), r=[xk_ + "_%d" % q_ for q_ in range(4)])
                for s4 in range(4):
                    ps, pk = K.psnext()
                    tp = ps.bitcast(BF16).rearrange("p (a b) -> p a b", a=8)
                    for kc in range(8):
                        K.tr(tp[:, kc, :], xs_[:, s4, kc * 128:(kc + 1) * 128], ident_bf[:], r=[xk_ + "_%d" % s4], w=[pk])
                    K.copy(xsT[:, :, s4 * 128:(s4 + 1) * 128], tp, r=[pk], w=[xTk], eng="scalar" if s4 % 2 else "vector")
                xsTs[e] = (xsT, xTk)

            K.reserved = {7}
            cbank, cbk = K.bank(7)
            gather(0)
            transp(0)
            for e in range(NE):
                if e + 1 < NE:
                    gather(e + 1)
                xsT, xTk = xsTs.pop(e)
                hid, hk = hidr.next()
                for c in range(11):
                    issue_to(pos_of[("u", e, c)] + 6)
                    wgb, wgk = issued[("g", e, c)]
                    wub, wuk = issued[("u", e, c)]
                    for fbi in range(2):
                        fb = c * 2 + fbi
                        pg, pgk = K.psnext()
                        for kc in range(8):
                            K.mm(pg, wgb[:, kc, fbi * 128:(fbi + 1) * 128], xsT[:, kc, :], kc == 0, kc == 7, r=[wgk, xTk], w=[pgk])
                        pu, puk = K.psnext()
                        for kc in range(8):
                            K.mm(pu, wub[:, kc, fbi * 128:(fbi + 1) * 128], xsT[:, kc, :], kc == 0, kc == 7, r=[wuk, xTk], w=[puk])
                        sgt, sgk = sgtr.next()
                        K.act(sgt[:], pg, AF.Silu, r=[pgk], w=[sgk])
                        K.tt(hid[:, fb, :], sgt[:], pu, ALU.mult, r=[sgk, puk], w=[hk])
                    if e + 2 < NE:
                        if c > 0:
                            comp_steps(e + 2, 3 * (c - 1), 3 * c, cbank, cbk)
                        comp_sel(e + 2, 3 * c, min(3 * c + 3, 32))
                if e + 2 < NE:
                    comp_steps(e + 2, 30, 32, cbank, cbk)
                if e + 1 < NE:
                    issue_to(pos_of[("u", e + 1, 3)])
                if debug and e == 0:
                    K.dma("sync", dbg["hid0"], hid[:].rearrange("p a b -> p (a b)"), r=[hk])
                if e + 1 < NE:
                    transp(e + 1)
                for cg in range(2):
                    issue_until(("d", e, cg))
                    wdb, wdk = issued[("d", e, cg)]
                    for s4 in range(4):
                        col = e * 4 + s4
                        py, pyk = K.psnext()
                        for fb in range(NFB):
                            K.mm(py, hid[:, fb, s4 * 128:(s4 + 1) * 128], wdb[:, fb, :], fb == 0, fb == NFB - 1, r=[hk, wdk], w=[pyk])
                        ysg, ysk = ysgr.next()
                        K.stt(ysg[:], py, gate_all[:, col:col + 1], GF[:, cg * 512:(cg + 1) * 512], ALU.mult, ALU.mult, r=[pyk, "gate%d" % col], w=[ysk])
                        if debug and col == 0 and cg == 0:
                            K.dma("sync", dbg["ysg0"], ysg[:], r=[ysk])
                        S.add("gpsimd", (lambda ysg=ysg, cg=cg, col=col: lambda g: g.indirect_dma_start(
                            out=x2_d[cg], out_offset=bass.IndirectOffsetOnAxis(ap=idx_all[:, col:col + 1], axis=0),
                            in_=ysg[:], in_offset=None, compute_op=ALU.add))(),
                            r=[ysk, "idx%d" % col] + ["x2_%d_%d_%d" % (cg, (e + 1) % 2, q_) for q_ in range(4)],
                            w=["x2_%d_%d_%d" % (cg, e % 2, s4)], dma=True)
            S.emit()

        K.reserved = set()
        late.close()
        with contextlib.ExitStack() as st:
            gfin = sb(st, "gfin", [128, D], F32)
            K.dma("sync", gfin[:], gvecs_b[:, 2 * D:3 * D], w=["gfin"])
            xr = Ring(st, nc, "x7", 3, [128, D], F32)
            jr = Ring(st, nc, "j7", 2, [128, D], BF16)
            orr = Ring(st, nc, "o7", 3, [128, D], F32)
            s7 = sb(st, "s7", [128, 3 * NT], F32)
            K.memset(s7[:], 0.0, w=["s7_%d" % i for i in range(NT)])
            for tile in range(NT):
                rows = slice(tile * 128, (tile + 1) * 128)
                xt, xk = xr.next()
                K.dma("gpsimd", xt[:, 0:512], x2_d[0][rows, :], w=[xk + "a"])
                K.dma("gpsimd", xt[:, 512:1024], x2_d[1][rows, :], w=[xk + "b"])
                jt, jk = jr.next()
                sk = "s7_%d" % tile
                K.act(jt[:], xt[:], AF.Square, r=[xk + "a", xk + "b"], w=[jk, sk], accum_out=s7[:, tile:tile + 1])
                K.act(s7[:, NT + tile:NT + tile + 1], s7[:, tile:tile + 1], AF.Sqrt, r=[sk], w=[sk], bias=eps_t[:], scale=1.0 / D)
                K.recip(s7[:, 2 * NT + tile:2 * NT + tile + 1], s7[:, NT + tile:NT + tile + 1], r=[sk], w=[sk])
                ot, ok = orr.next()
                K.stt(ot[:], xt[:], s7[:, 2 * NT + tile:2 * NT + tile + 1], gfin[:], ALU.mult, ALU.mult, r=[xk + "a", xk + "b", sk, "gfin"], w=[ok])
                K.dma("sync", out[rows, :], ot[:], r=[ok])
            S.emit()
    return nc


def _blk(W, nb):
    Kd, N = W.shape
    return np.ascontiguousarray(W.reshape(8, 128, N // nb, nb).transpose(2, 1, 0, 3)).reshape(N // nb, 128, 8 * nb)


def _pcol(v):
    return np.ascontiguousarray(v.reshape(8, 128).T)


def _rope_tables():
    inv = (10000.0 ** (-np.arange(16, dtype=np.float32) / 16)).astype(np.float32)
    t = np.arange(T)
    row = (t // 64).astype(np.float32)
    col = (t % 64).astype(np.float32)
    C = np.zeros((128, T), np.float32)
    Sg = np.zeros((128, T), np.float32)
    for p in range(128):
        d = p % 64
        posv = row if d < 32 else col
        dd = d % 32
        j = dd % 16
        half = dd // 16
        ang = (posv * inv[j]).astype(np.float32)
        C[p] = np.cos(ang)
        Sg[p] = np.sin(ang) * (-1.0 if half == 0 else 1.0)
    return C, Sg


def _rot_perm():
    perm = np.zeros(1024, np.int64)
    for c in range(1024):
        base = (c // 64) * 64
        d = c % 64
        dd = d % 32
        half = dd // 16
        perm[c] = base + (d + 16 if half == 0 else d - 16)
    return perm


def prep_shared(inp, upto=99):
    f = np.float32
    sh = {}
    w_ada = np.asarray(inp["w_ada"], f)[0]
    sh["w_ada_l"] = _blk(w_ada, 512)
    sh["bada_b"] = np.ascontiguousarray(np.broadcast_to(np.asarray(inp["b_ada"], f)[0][None, :], (128, 6 * D)))
    gv = np.concatenate([np.asarray(inp["g_norm_mix"], f)[0], np.asarray(inp["g_norm_ffn"], f)[0], np.asarray(inp["g_final"], f)])
    sh["gvecs_b"] = np.ascontiguousarray(np.broadcast_to(gv[None, :], (128, 3 * D)))
    wdw = np.asarray(inp["w_dw"], f)[0]
    wdw_l = np.ascontiguousarray(wdw.T.reshape(8, 128, 31).transpose(1, 0, 2)).reshape(128, 8 * 31)
    sh["pvecs"] = np.ascontiguousarray(np.concatenate([
        _pcol(np.asarray(inp["b_dw"], f)[0]), _pcol(np.asarray(inp["ln_g_conv"], f)[0]),
        _pcol(np.asarray(inp["ln_b_conv"], f)[0]), _pcol(np.asarray(inp["b_conv_out"], f)[0]), wdw_l], axis=1))
    lam = np.concatenate([np.asarray(inp[k], f)[0] for k in ("lambda_q1", "lambda_k1", "lambda_q2", "lambda_k2")])
    sh["lamv_b"] = np.ascontiguousarray(np.broadcast_to(lam[None, :], (128, 256)))
    sh["gsub_b"] = np.ascontiguousarray(np.broadcast_to(np.asarray(inp["g_subln"], f)[0][None, :], (128, 128)))
    cf = np.zeros((128, 801), f)
    cf[:, 0:128] = np.eye(128, dtype=f)
    cf[:, 128:640] = np.arange(512, dtype=f)[None, :]
    cf[:, 640:768] = np.triu(np.ones((128, 128), f))
    cf[:, 768] = np.arange(128, dtype=f)
    cf[:, 769:801] = np.arange(32, dtype=f)[None, :]
    sh["constf"] = cf
    C, Sg = _rope_tables()
    sh["ropeC"], sh["ropeS"] = C, Sg
    w_in = np.asarray(inp["w_in"], f)[0]
    a, b, q, k, v, gc, ga = [w_in[:, i * 1024:(i + 1) * 1024] for i in range(7)]
    perm = _rot_perm()
    qr, kr = q[:, perm], k[:, perm]
    blocks = []
    ab, bb, gcb, gab = _blk(a, 128), _blk(b, 128), _blk(gc, 128), _blk(ga, 128)
    for j in range(8):
        blocks += [ab[j], bb[j], gcb[j], gab[j]]
    qb, qrb, kb, krb, vb = _blk(q, 128), _blk(qr, 128), _blk(k, 128), _blk(kr, 128), _blk(v, 128)
    for h in range(8):
        blocks += [qb[h], qrb[h], kb[h], krb[h], vb[h]]
    sh["w_fm"] = np.ascontiguousarray(np.stack(blocks))

    def plain(W):
        return np.ascontiguousarray(W.reshape(8, 128, W.shape[1]).transpose(1, 0, 2)).reshape(128, 8 * W.shape[1])

    sh["w3"] = np.ascontiguousarray(np.stack([plain(np.asarray(inp["w_conv_out"], f)[0]),
                                              plain(np.asarray(inp["w_attn_out"], f)[0]),
                                              plain(np.asarray(inp["w_out"], f)[0])]))
    sh["w_router_l"] = plain(np.asarray(inp["w_router"], f)[0])
    if upto >= 6:
        wg = np.asarray(inp["w_expert_gate"], f)[0]
        wu = np.asarray(inp["w_expert_up"], f)[0]
        wd = np.asarray(inp["w_expert_down"], f)[0]

        def gl(W):
            return np.ascontiguousarray(W.reshape(NE, 8, 128, 11, 256).transpose(0, 3, 2, 1, 4)).reshape(NE, 11, 128, 2048)

        sh["wg_l"] = gl(wg)
        sh["wu_l"] = gl(wu)
        sh["wd_l"] = np.ascontiguousarray(wd.reshape(NE, NFB, 128, 2, 512).transpose(0, 3, 2, 1, 4)).reshape(NE, 2, 128, NFB * 512)
    return sh


def prep_core(inp, b):
    f = np.float32
    cv = np.concatenate([_pcol(np.asarray(inp["c"], f)[b]), _pcol(np.asarray(inp["c_ctx"], f))], axis=1)
    return {"x": np.ascontiguousarray(np.asarray(inp["x"], f)[b]),
            "ctx": np.ascontiguousarray(np.asarray(inp["ctx"], f)[b]),
            "cvec": np.ascontiguousarray(cv)}


_CACHE = {}


def kernel(**inputs):
    if "nc" not in _CACHE:
        _CACHE["nc"] = build()
    nc = _CACHE["nc"]
    sh = prep_shared(inputs)
    in_maps = []
    for b in range(8):
        m = dict(sh)
        m.update(prep_core(inputs, b))
        in_maps.append(m)
    res = run_bass_kernel_spmd(nc, in_maps, core_ids=list(range(8)))
    return np.stack([np.asarray(r["out"], np.float32) for r in res.results], axis=0)
```

```python
import contextlib
import math

import numpy as np
import concourse.bass as bass
import concourse.mybir as mybir
from concourse.bass_utils import run_bass_kernel_spmd

F32 = mybir.dt.float32
BF16 = mybir.dt.bfloat16
I32 = mybir.dt.int32
AF = mybir.ActivationFunctionType
ALU = mybir.AluOpType
AX = mybir.AxisListType

D = 1024
T = 4096
TC = 256
TK = T + TC
NT = T // 128
NKT = TK // 128
NE = 16
CAP = 512
DFF = 2816
NFB = DFF // 128
EPS = 1e-6
LAM_INIT = 0.8 - 0.6 * math.exp(-0.3 * 0)
ENGINES = ("sync", "scalar", "vector", "gpsimd", "tensor")
NBIS = 30


class Op:
    __slots__ = ("eng", "fn", "is_dma", "deps", "signal", "count", "dsem", "dval", "idx")


class Sched:
    def __init__(self, nc, stack, n_dma_sems=8):
        self.nc = nc
        self.n_dma_sems = n_dma_sems
        self.esem = {e: stack.enter_context(nc.semaphore("p_" + e)) for e in ENGINES}
        self.dsem = {}
        for e in ("sync", "scalar", "gpsimd"):
            for k in range(n_dma_sems):
                self.dsem[(e, k)] = stack.enter_context(nc.semaphore("d_%s_%d" % (e, k)))
        self.ecount = {e: 0 for e in ENGINES}
        self.dma_uses = {k: 0 for k in self.dsem}
        self.dma_rr = {e: 0 for e in ENGINES}
        self._reset()

    def _reset(self):
        self.ops = {e: [] for e in ENGINES}
        self.last_w = {}
        self.readers = {}
        self.dma_last = {}

    def add(self, eng, fn, r=(), w=(), dma=False):
        op = Op()
        op.eng, op.fn, op.is_dma = eng, fn, dma
        op.deps = []
        op.signal = False
        op.count = None
        op.dsem = None
        op.dval = None
        op.idx = len(self.ops[eng])
        deps = set()
        for k in r:
            lw = self.last_w.get(k)
            if lw is not None:
                deps.add(lw)
        for k in w:
            lw = self.last_w.get(k)
            if lw is not None:
                deps.add(lw)
            for rd in self.readers.get(k, ()):
                deps.add(rd)
        for d in deps:
            if d.eng == "tensor" and eng == "tensor" and not d.is_dma and not dma:
                continue
            op.deps.append(d)
        if dma:
            k = self.dma_rr[eng]
            self.dma_rr[eng] = (k + 1) % self.n_dma_sems
            key = (eng, k)
            self.dma_uses[key] += 1
            op.dsem = key
            op.dval = 16 * self.dma_uses[key]
            prev = self.dma_last.get(key)
            if prev is not None:
                op.deps.append(prev)
            self.dma_last[key] = op
        for k in r:
            self.readers.setdefault(k, []).append(op)
        for k in w:
            self.last_w[k] = op
            self.readers[k] = []
        self.ops[eng].append(op)
        return op

    def emit(self):
        nc = self.nc
        for e in ENGINES:
            for op in self.ops[e]:
                best = {}
                keep = []
                for d in op.deps:
                    if d.is_dma:
                        keep.append(d)
                        continue
                    b = best.get(d.eng)
                    if b is None or d.idx > b.idx:
                        best[d.eng] = d
                for d in best.values():
                    d.signal = True
                    keep.append(d)
                op.deps = keep
        for e in ENGINES:
            for op in self.ops[e]:
                if not op.is_dma and op.signal:
                    self.ecount[e] += 1
                    op.count = self.ecount[e]
        finals = list(self.dma_last.values())
        ops = self.ops
        esem, dsem = self.esem, self.dsem

        def target(d):
            if d.is_dma:
                return dsem[d.dsem], d.dval
            return esem[d.eng], d.count

        def body(ename):
            def run(eng):
                waited = {}
                for op in ops[ename]:
                    need = {}
                    for d in op.deps:
                        s, v = target(d)
                        if need.get(id(s), (None, 0))[1] < v:
                            need[id(s)] = (s, v)
                    for sid, (s, v) in need.items():
                        if waited.get(sid, 0) >= v:
                            continue
                        eng.wait_ge(s, v)
                        waited[sid] = v
                    ins = op.fn(eng)
                    if op.is_dma:
                        ins.then_inc(dsem[op.dsem], 16)
                    elif op.signal:
                        ins.then_inc(esem[ename], 1)
                for d in finals:
                    if d.eng != ename:
                        continue
                    s, v = target(d)
                    if waited.get(id(s), 0) >= v:
                        continue
                    eng.wait_ge(s, v)
                    waited[id(s)] = v

            return run

        with nc.Block() as block:
            for e in ENGINES:
                if not ops[e]:
                    continue
                getattr(block, e)(body(e))
        self._reset()


class Ring:
    def __init__(self, stack, nc, name, n, shape, dt):
        self.t = [stack.enter_context(nc.sbuf_tensor("%s%d" % (name, i), list(shape), dt)) for i in range(n)]
        self.keys = ["%s%d" % (name, i) for i in range(n)]
        self.i = 0

    def next(self):
        k = self.i % len(self.t)
        self.i += 1
        return self.t[k], self.keys[k]


class KB:
    def __init__(self, nc, S, PS):
        self.nc, self.S, self.PS = nc, S, PS
        self.psi = 0
        self.reserved = set()

    def bank(self, b):
        return self.PS[b // 2][:, (b % 2) * 512:(b % 2 + 1) * 512], "ps%d" % b

    def psnext(self):
        b = self.psi % 8
        self.psi += 1
        while b in self.reserved:
            b = self.psi % 8
            self.psi += 1
        return self.bank(b)

    def pspair(self):
        if self.psi % 2:
            self.psi += 1
        p = (self.psi % 8) // 2
        self.psi += 2
        return self.PS[p], ["ps%d" % (2 * p), "ps%d" % (2 * p + 1)]

    def dma(self, q, out, in_, r=(), w=()):
        return self.S.add(q, lambda e: e.dma_start(out=out, in_=in_), r=r, w=w, dma=True)

    def mm(self, out, lhsT, rhs, start, stop, r=(), w=()):
        return self.S.add("tensor", lambda e: e.matmul(out, lhsT, rhs, start=start, stop=stop), r=r, w=w)

    def tr(self, out, in_, ident, r=(), w=()):
        return self.S.add("tensor", lambda e: e.transpose(out, in_, ident), r=r, w=w)

    def act(self, out, in_, func, r=(), w=(), bias=None, scale=None, accum_out=None):
        kw = {}
        if bias is not None:
            kw["bias"] = bias
        if scale is not None:
            kw["scale"] = scale
        if accum_out is not None:
            kw["accum_out"] = accum_out
        return self.S.add("scalar", lambda e: e.activation(out=out, in_=in_, func=func, **kw), r=r, w=w)

    def tt(self, out, in0, in1, op, r=(), w=(), eng="vector"):
        return self.S.add(eng, lambda e: e.tensor_tensor(out=out, in0=in0, in1=in1, op=op), r=r, w=w)

    def ts(self, out, in0, s1, op0, r=(), w=(), s2=None, op1=None, eng="vector"):
        if op1 is None:
            return self.S.add(eng, lambda e: e.tensor_scalar(out=out, in0=in0, scalar1=s1, scalar2=None, op0=op0), r=r, w=w)
        return self.S.add(eng, lambda e: e.tensor_scalar(out=out, in0=in0, scalar1=s1, scalar2=s2, op0=op0, op1=op1), r=r, w=w)

    def stt(self, out, in0, scalar, in1, op0, op1, r=(), w=(), accum_out=None):
        if accum_out is None:
            return self.S.add("vector", lambda e: e.scalar_tensor_tensor(out=out, in0=in0, scalar=scalar, in1=in1, op0=op0, op1=op1), r=r, w=w)
        return self.S.add("vector", lambda e: e.scalar_tensor_tensor(out=out, in0=in0, scalar=scalar, in1=in1, op0=op0, op1=op1, accum_out=accum_out), r=r, w=w)

    def copy(self, out, in_, r=(), w=(), eng="vector"):
        if eng == "scalar":
            return self.S.add("scalar", lambda e: e.copy(out=out, in_=in_), r=r, w=w)
        return self.S.add(eng, lambda e: e.tensor_copy(out=out, in_=in_), r=r, w=w)

    def recip(self, out, in_, r=(), w=()):
        return self.S.add("vector", lambda e: e.reciprocal(out=out, in_=in_), r=r, w=w)

    def memset(self, out, val, r=(), w=(), eng="vector"):
        return self.S.add(eng, lambda e: e.memset(out, val), r=r, w=w)

    def reduce(self, out, in_, op, r=(), w=()):
        return self.S.add("vector", lambda e: e.tensor_reduce(out=out, in_=in_, axis=AX.X, op=op), r=r, w=w)


def build(upto=99, debug=False):
    nc = bass.Bass("TRN2", target_bir_lowering=False)

    def din(name, shape, dt=F32):
        return nc.dram_tensor(name, list(shape), dt, kind="ExternalInput").ap()

    def dscr(name, shape, dt):
        return nc.dram_tensor(name, list(shape), dt, kind="ExternalOutput" if debug else "Internal").ap()

    x = din("x", [T, D])
    ctx = din("ctx", [TC, D])
    cvec = din("cvec", [128, 16])
    w_ada_l = din("w_ada_l", [12, 128, 8 * 512])
    bada_b = din("bada_b", [128, 6 * D])
    gvecs_b = din("gvecs_b", [128, 3 * D])
    pvecs = din("pvecs", [128, 32 + 8 * 31])
    lamv_b = din("lamv_b", [128, 256])
    gsub_b = din("gsub_b", [128, 128])
    constf = din("constf", [128, 801])
    ropeC = din("ropeC", [128, T])
    ropeS = din("ropeS", [128, T])
    w_fm = din("w_fm", [72, 128, 1024])
    w3 = din("w3", [3, 128, 8 * 1024])
    w_router_l = din("w_router_l", [128, 8 * 16])
    if upto >= 6:
        wg_l = din("wg_l", [NE, 11, 128, 2048])
        wu_l = din("wu_l", [NE, 11, 128, 2048])
        wd_l = din("wd_l", [NE, 2, 128, NFB * 512])
    out = nc.dram_tensor("out", [T, D], F32, kind="ExternalOutput").ap()

    y_d = dscr("y_d", [8, 128, T], F32)
    sgc_d = dscr("sgc_d", [8, 128, T], BF16)
    sga_d = dscr("sga_d", [8, 128, T], BF16)
    o_d = dscr("o_d", [T, D], BF16)
    x2_d = [dscr("x2a_d", [T, 512], F32), dscr("x2b_d", [T, 512], F32)]
    hf_d = dscr("hf_d", [T, D], BF16)
    dbg = {}
    if debug:
        dbg["modL"] = dscr("dbg_modL", [128, 6 * D], F32)
        dbg["hT"] = dscr("dbg_hT", [128, 8 * TK], BF16)
        dbg["sso"] = dscr("dbg_sso", [128, 256], F32)
        dbg["lg"] = dscr("dbg_lg", [128, 512], F32)
        dbg["aff"] = dscr("dbg_aff", [128, 512], F32)
        dbg["pos"] = dscr("dbg_pos", [128, 512], F32)
        dbg["idx"] = dscr("dbg_idx", [128, 64], I32)
        dbg["gate"] = dscr("dbg_gate", [128, 64], F32)
        dbg["xs0"] = dscr("dbg_xs0", [128, 4 * D], BF16)
        dbg["hid0"] = dscr("dbg_hid0", [128, NFB * 512], BF16)
        dbg["ysg0"] = dscr("dbg_ysg0", [128, 512], F32)

    with contextlib.ExitStack() as top:
        S = Sched(nc, top)
        PS = [top.enter_context(nc.psum_tensor("ps%d" % i, [128, 1024], F32)) for i in range(4)]
        K = KB(nc, S, PS)

        def sb(stack, name, shape, dt):
            return stack.enter_context(nc.sbuf_tensor(name, list(shape), dt))

        cf = sb(top, "cf", [128, 801], F32)
        ident_f = cf[:, 0:128]
        iota_row = cf[:, 128:640]
        pidx = cf[:, 768:769]
        tileidx = cf[:, 769:801]
        ident_bf = sb(top, "ident_bf", [128, 128], BF16)
        tri_bf = sb(top, "tri_bf", [128, 128], BF16)
        ones_bf = sb(top, "ones_bf", [128, 128], BF16)
        ones_f = sb(top, "ones_f", [128, 128], F32)
        eps_t = sb(top, "eps_t", [128, 1], F32)
        modL = sb(top, "modL", [128, 6 * D], F32)
        SH1, G1, GM = modL[:, 0:D], modL[:, D:2 * D], modL[:, 2 * D:3 * D]
        SH2, G2, GF = modL[:, 3 * D:4 * D], modL[:, 4 * D:5 * D], modL[:, 5 * D:6 * D]
        pv = sb(top, "pv", [128, 32 + 8 * 31], F32)
        neg_lam = sb(top, "neg_lam", [128, 1], F32)
        gsub08 = sb(top, "gsub08", [128, 128], F32)
        ss_o = sb(top, "ss_o", [128, 256], F32)
        lg = sb(top, "lg", [128, 32, 16], F32)
        idx_all = sb(top, "idx_all", [128, 64], I32)
        gate_all = sb(top, "gate_all", [128, 64], F32)

        mid = contextlib.ExitStack()
        hT = sb(mid, "hT", [128, 8, TK], BF16)
        midc = contextlib.ExitStack()
        modC = sb(midc, "modC", [128, 2 * D], F32)

        with contextlib.ExitStack() as st:
            K.dma("sync", cf[:], constf, w=["cf"])
            K.dma("sync", pv[:], pvecs, w=["pv"])
            cv = sb(st, "cv", [128, 16], F32)
            K.dma("sync", cv[:], cvec, w=["cv"])
            bada = sb(st, "bada", [128, 6 * D], F32)
            K.dma("sync", bada[:], bada_b, w=["bada"])
            gv = sb(st, "gv", [128, 2 * D], F32)
            K.dma("sync", gv[:], gvecs_b[:, 0:2 * D], w=["gv"])
            lv = sb(st, "lv", [128, 256], F32)
            K.dma("sync", lv[:], lamv_b, w=["lv"])
            gs = sb(st, "gs", [128, 128], F32)
            K.dma("sync", gs[:], gsub_b, w=["gs"])

            K.copy(ident_bf[:], ident_f, r=["cf"], w=["ident_bf"])
            K.copy(tri_bf[:], cf[:, 640:768], r=["cf"], w=["tri_bf"])
            K.memset(ones_bf[:], 1.0, w=["ones_bf"])
            K.memset(ones_f[:], 1.0, w=["ones_f"])
            K.memset(eps_t[:], EPS, w=["eps_t"])
            K.memset(ss_o[:], 0.0, w=["ss_o"])
            K.ts(gsub08[:], gs[:], 1.0 - LAM_INIT, ALU.mult, r=["gs"], w=["gsub08"])

            pr = sb(st, "pr", [128, 128], F32)
            K.tt(pr[:, 0:64], lv[:, 0:64], lv[:, 64:128], ALU.mult, r=["lv"], w=["pr"])
            K.tt(pr[:, 64:128], lv[:, 128:192], lv[:, 192:256], ALU.mult, r=["lv", "pr"], w=["pr"])
            sl = sb(st, "sl", [128, 2], F32)
            K.reduce(sl[:], pr[:].rearrange("p (a b) -> p a b", a=2), ALU.add, r=["pr"], w=["sl"])
            el = sb(st, "el", [128, 2], F32)
            K.act(el[:], sl[:], AF.Exp, r=["sl"], w=["el"])
            dl = sb(st, "dl", [128, 1], F32)
            K.tt(dl[:], el[:, 1:2], el[:, 0:1], ALU.subtract, r=["el"], w=["dl"])
            K.ts(neg_lam[:], dl[:], -LAM_INIT, ALU.add, r=["dl"], w=["neg_lam"])

            sc = sb(st, "sc", [128, 16], F32)
            K.act(sc[:], cv[:], AF.Silu, r=["cv"], w=["sc"])
            rep = sb(st, "rep", [128, 16, 128], BF16)
            for kc in range(16):
                K.ts(rep[:, kc, :], ones_f[:], sc[:, kc:kc + 1], ALU.mult, r=["ones_f", "sc"], w=["rep"])
            wring = Ring(st, nc, "wada", 2, [128, 8, 512], BF16)
            for g in range(12):
                wb, wk = wring.next()
                K.dma("gpsimd", wb[:], w_ada_l[g].rearrange("p (k f) -> p k f", k=8), w=[wk])
                ps, pk = K.psnext()
                for kc in range(8):
                    K.mm(ps, rep[:, kc, :], wb[:, kc, :], kc == 0, kc == 7, r=["rep", wk], w=[pk])
                K.tt(modL[:, g * 512:(g + 1) * 512], ps, bada[:, g * 512:(g + 1) * 512], ALU.add,
                     r=[pk, "bada"], w=["modL%d" % g])
                if g < 4:
                    ps2, pk2 = K.psnext()
                    for kc in range(8):
                        K.mm(ps2, rep[:, 8 + kc, :], wb[:, kc, :], kc == 0, kc == 7, r=["rep", wk], w=[pk2])
                    K.tt(modC[:, g * 512:(g + 1) * 512], ps2, bada[:, g * 512:(g + 1) * 512], ALU.add,
                         r=[pk2, "bada"], w=["modC%d" % g])
            allL = ["modL%d" % g for g in range(12)]
            allC = ["modC%d" % g for g in range(4)]
            K.ts(G1, G1, 1.0, ALU.add, r=allL, w=allL)
            K.tt(G1, G1, gv[:, 0:D], ALU.mult, r=allL + ["gv"], w=allL)
            K.ts(G2, G2, 1.0, ALU.add, r=allL, w=allL)
            K.tt(G2, G2, gv[:, D:2 * D], ALU.mult, r=allL + ["gv"], w=allL)
            K.ts(modC[:, D:2 * D], modC[:, D:2 * D], 1.0, ALU.add, r=allC, w=allC)
            K.tt(modC[:, D:2 * D], modC[:, D:2 * D], gv[:, 0:D], ALU.mult, r=allC + ["gv"], w=allC)
            if debug:
                K.dma("sync", dbg["modL"], modL[:], r=allL)
            S.emit()
        if upto < 1:
            return nc

        with contextlib.ExitStack() as st:
            xr = Ring(st, nc, "xt", 3, [128, D], F32)
            t1r = Ring(st, nc, "t1_", 2, [128, D], F32)
            hbr = Ring(st, nc, "hb", 2, [128, D], BF16)
            jr = Ring(st, nc, "junk", 2, [128, D], BF16)
            ss = sb(st, "ss", [128, 3 * NKT], F32)
            K.memset(ss[:], 0.0, w=["ss%d" % i for i in range(NKT)])
            for i in range(NKT):
                src = x[i * 128:(i + 1) * 128, :] if i < NT else ctx[(i - NT) * 128:(i - NT + 1) * 128, :]
                xt, xk = xr.next()
                K.dma("sync", xt[:], src, w=[xk])
                jk_t, jk = jr.next()
                sk = "ss%d" % i
                K.act(jk_t[:], xt[:], AF.Square, r=[xk], w=[jk, sk], accum_out=ss[:, i:i + 1])
                K.act(ss[:, NKT + i:NKT + i + 1], ss[:, i:i + 1], AF.Sqrt, r=[sk], w=[sk], bias=eps_t[:], scale=1.0 / D)
                K.recip(ss[:, 2 * NKT + i:2 * NKT + i + 1], ss[:, NKT + i:NKT + i + 1], r=[sk], w=[sk])
                Gm, SHm = (G1, SH1) if i < NT else (modC[:, D:2 * D], modC[:, 0:D])
                t1, tk = t1r.next()
                K.stt(t1[:], xt[:], ss[:, 2 * NKT + i:2 * NKT + i + 1], Gm, ALU.mult, ALU.mult, r=[xk, sk], w=[tk])
                hb, hk = hbr.next()
                K.tt(hb[:], t1[:], SHm, ALU.add, r=[tk], w=[hk])
                ps, pk = K.psnext()
                tp = ps.bitcast(BF16).rearrange("p (a b) -> p a b", a=8)
                for kc in range(8):
                    K.tr(tp[:, kc, :], hb[:, kc * 128:(kc + 1) * 128], ident_bf[:], r=[hk], w=[pk])
                K.copy(hT[:, :, i * 128:(i + 1) * 128], tp, r=[pk], w=["hT%d" % i], eng="scalar" if i % 2 else "vector")
            if debug:
                K.dma("sync", dbg["hT"], hT[:].rearrange("p a b -> p (a b)"), r=["hT%d" % i for i in range(NKT)])
            S.emit()
        midc.close()
        if upto < 2:
            mid.close()
            return nc

        with contextlib.ExitStack() as st:
            wr_ = Ring(st, nc, "wblk", 2, [128, 4, 1024], BF16)
            ur = Ring(st, nc, "u", 2, [128, T + 30], BF16)
            dgr = Ring(st, nc, "dg", 2, [128, 31, 128], BF16)
            sgr = Ring(st, nc, "sg", 2, [128, 512], F32)
            gcr = Ring(st, nc, "gcc", 3, [128, 512], BF16)
            gar = Ring(st, nc, "gac", 3, [128, 512], BF16)
            ycr = Ring(st, nc, "yc", 3, [128, 512], F32)
            for ut, uk in zip(ur.t, ur.keys):
                K.memset(ut[:, 0:15], 0.0, w=[uk])
                K.memset(ut[:, T + 15:T + 30], 0.0, w=[uk])

            def conv(j, ub, uk, dgb, dk):
                for n in range(8):
                    pc, pk = K.psnext()
                    for k in range(31):
                        K.mm(pc, dgb[:, k, :], ub[:, n * 512 + k:n * 512 + k + 512], k == 0, k == 30, r=[dk, uk], w=[pk])
                    yc, yk = ycr.next()
                    K.act(yc[:], pc, AF.Identity, r=[pk], w=[yk], bias=pv[:, j:j + 1])
                    K.dma("sync", y_d[j, :, n * 512:(n + 1) * 512], yc[:], r=[yk])

            pend = None
            for j in range(8):
                wb, wk = wr_.next()
                K.dma("gpsimd", wb[:], w_fm[j * 4:(j + 1) * 4].rearrange("b p f -> p b f"), w=[wk])
                ub, uk = ur.next()
                for n in range(8):
                    cs = slice(n * 512, (n + 1) * 512)
                    pss = []
                    for b in range(4):
                        ps, pk = K.psnext()
                        for kc in range(8):
                            K.mm(ps, wb[:, b, kc * 128:(kc + 1) * 128], hT[:, kc, cs], kc == 0, kc == 7, r=[wk], w=[pk])
                        pss.append((ps, pk))
                    sg, sk = sgr.next()
                    K.act(sg[:], pss[1][0], AF.Sigmoid, r=[pss[1][1]], w=[sk])
                    K.tt(ub[:, 15 + n * 512:15 + (n + 1) * 512], pss[0][0], sg[:], ALU.mult, r=[pss[0][1], sk], w=[uk])
                    gc, gk = gcr.next()
                    K.act(gc[:], pss[2][0], AF.Sigmoid, r=[pss[2][1]], w=[gk])
                    K.dma("sync", sgc_d[j, :, cs], gc[:], r=[gk])
                    ga, gak = gar.next()
                    K.act(ga[:], pss[3][0], AF.Sigmoid, r=[pss[3][1]], w=[gak])
                    K.dma("sync", sga_d[j, :, cs], ga[:], r=[gak])
                dgb, dk = dgr.next()
                for k in range(31):
                    K.ts(dgb[:, k, :], ident_bf[:], pv[:, 32 + j * 31 + k:32 + j * 31 + k + 1], ALU.mult, w=[dk])
                if pend is not None:
                    conv(*pend)
                pend = (j, ub, uk, dgb, dk)
            conv(*pend)
            S.emit()
        if upto < 3:
            mid.close()
            return nc

        with contextlib.ExitStack() as st:
            rC = sb(st, "rC", [128, T], F32)
            rS = sb(st, "rS", [128, T], F32)
            K.dma("sync", rC[:], ropeC, w=["rC"])
            K.dma("sync", rS[:], ropeS, w=["rS"])
            hwr = Ring(st, nc, "hw", 1, [128, 5, 1024], BF16)
            qTr = Ring(st, nc, "qT", 1, [128, 2, T], BF16)
            kTr = Ring(st, nc, "kT", 1, [128, TK], BF16)
            Var = Ring(st, nc, "Va", 1, [128, NKT, 129], BF16)
            ostr = Ring(st, nc, "ost", 1, [128, NT, 128], BF16)
            t1r = Ring(st, nc, "ra", 2, [128, 512], F32)
            t2r = Ring(st, nc, "rb", 2, [128, 512], F32)
            PTr = Ring(st, nc, "PT", 3, [128, 8, 128], BF16)
            o1r = Ring(st, nc, "o1", 2, [128, 128], F32)
            odr = Ring(st, nc, "od", 2, [128, 128], F32)
            jr = Ring(st, nc, "jk", 2, [128, 128], F32)
            rvr = Ring(st, nc, "rv", 4, [128, 2], F32)
            for vt, vk in zip(Var.t, Var.keys):
                K.memset(vt[:, :, 128:129], 1.0, w=[vk])
            qbr = Ring(st, nc, "qb", 4, [128, 512], BF16)
            Pm = sb(st, "Pm", [128, 128], BF16)
            Pm4 = Pm[:].rearrange("p (m h c) -> p m h c", h=2, c=16)
            id4 = ident_bf[:].rearrange("p (m h c) -> p m h c", h=2, c=16)
            K.copy(Pm4[:, :, 0, :], id4[:, :, 1, :], w=["Pm"])
            K.copy(Pm4[:, :, 1, :], id4[:, :, 0, :], r=["Pm"], w=["Pm"])
            for qt_, qk_ in zip(qTr.t, qTr.keys):
                K.memset(qt_[64:128, 0, :], 0.0, w=[qk_])
                K.memset(qt_[0:64, 1, :], 0.0, w=[qk_])
            groups = [(k0, min(k0 + 8, NKT)) for k0 in range(0, NKT, 8)]
            for h in range(8):
                hw, hwk = hwr.next()
                K.dma("gpsimd", hw[:], w_fm[32 + h * 5:32 + (h + 1) * 5].rearrange("b p f -> p b f"), w=[hwk])
                qT, qk = qTr.next()
                kT, kk = kTr.next()
                Va, vk = Var.next()
                ost, ok = ostr.next()

                def proj(blk, cs, n):
                    ps, pk = K.psnext()
                    for kc in range(8):
                        K.mm(ps[:, 0:n], hw[:, blk, kc * 128:(kc + 1) * 128], hT[:, kc, cs], kc == 0, kc == 7, r=[hwk], w=[pk])
                    return ps, pk

                def finish_rope(item):
                    cs, lst = item
                    for (b0, dst, dk, p0, k0_, qb, qbk) in lst:
                        p1, k1_ = K.psnext()
                        K.mm(p1, Pm[:], qb[:], True, True, r=[qbk, "Pm"], w=[k1_])
                        ta, tak = t1r.next()
                        tb, tbk = t2r.next()
                        K.tt(ta[:], p0, rC[:, cs], ALU.mult, r=[k0_, "rC", qbk], w=[tak])
                        K.tt(tb[:], p1, rS[:, cs], ALU.mult, r=[k1_, "rS"], w=[tbk])
                        if b0 == 0:
                            K.tt(dst[0:64, 0, cs], ta[0:64, :], tb[0:64, :], ALU.add, r=[tak, tbk], w=[dk])
                            K.tt(dst[64:128, 1, cs], ta[64:128, :], tb[64:128, :], ALU.add, r=[tak, tbk], w=[dk])
                        else:
                            K.tt(dst[:, cs], ta[:], tb[:], ALU.add, r=[tak, tbk], w=[dk])

                pend_r = None
                for n in range(8):
                    cs = slice(n * 512, (n + 1) * 512)
                    lst = []
                    for (b0, dst, dk) in ((0, qT, qk), (2, kT, kk)):
                        p0, k0_ = proj(b0, cs, 512)
                        qb, qbk = qbr.next()
                        K.copy(qb[:], p0, r=[k0_], w=[qbk], eng="scalar")
                        lst.append((b0, dst, dk, p0, k0_, qb, qbk))
                    if pend_r is not None:
                        finish_rope(pend_r)
                    pend_r = (cs, lst)
                finish_rope(pend_r)
                pc_, pck = proj(2, slice(T, TK), 256)
                K.copy(kT[:, T:TK], pc_[:, 0:256], r=[pck], w=[kk])
                for g0 in range(0, NKT, 4):
                    g1 = min(g0 + 4, NKT)
                    ps, pk = K.psnext()
                    for ti in range(g0, g1):
                        for kc in range(8):
                            K.mm(ps[:, (ti - g0) * 128:(ti - g0 + 1) * 128], hT[:, kc, ti * 128:(ti + 1) * 128],
                                 hw[:, 4, kc * 128:(kc + 1) * 128], kc == 0, kc == 7, r=[hwk], w=[pk])
                    K.copy(Va[:, g0:g1, 0:128], ps[:, 0:(g1 - g0) * 128].rearrange("p (a b) -> p a b", b=128), r=[pk], w=[vk])

                units = [(qt, sub, gi, k0, k1) for qt in range(NT) for sub in range(2) for gi, (k0, k1) in enumerate(groups)]
                o1s = {}
                pts = {}

                def scbuf(u):
                    return PS[u % 3], ["ps%d" % (2 * (u % 3)), "ps%d" % (2 * (u % 3) + 1)]

                def emitS(u):
                    qt, sub, gi, k0, k1 = units[u]
                    prt = slice(sub * 64, (sub + 1) * 64)
                    qs = slice(qt * 128, (qt + 1) * 128)
                    sc_, sck = scbuf(u)
                    for kt in range(k0, k1):
                        K.mm(sc_[:, (kt - k0) * 128:(kt - k0 + 1) * 128], kT[:, kt * 128:(kt + 1) * 128], qT[:, sub, qs],
                             True, True, r=[kk, qk], w=sck)

                def emitExp(u):
                    qt, sub, gi, k0, k1 = units[u]
                    sc_, sck = scbuf(u)
                    pt, ptk = PTr.next()
                    ng = k1 - k0
                    K.act(pt[:, 0:ng, :].rearrange("p a b -> p (a b)"), sc_[:, 0:ng * 128], AF.Exp, r=sck, w=[ptk], scale=0.125)
                    pts[u] = (pt, ptk)

                def emitPV(u):
                    qt, sub, gi, k0, k1 = units[u]
                    pt, ptk = pts.pop(u)
                    ab = 6 + (qt * 2 + sub) % 2
                    acc, acck = K.bank(ab)
                    acc = acc[:, 0:129]
                    for kt in range(k0, k1):
                        K.mm(acc, pt[:, kt - k0, :], Va[:, kt, :], kt == 0, kt == NKT - 1, r=[ptk, vk], w=[acck])
                    if gi != len(groups) - 1:
                        return
                    if sub == 0:
                        o1s[qt] = o1r.next()
                    o1, o1k = o1s[qt]
                    rv, rvk = rvr.next()
                    K.recip(rv[:, 0:1], acc[:, 128:129], r=[acck], w=[rvk])
                    if sub == 0:
                        K.ts(o1[:], acc[:, 0:128], rv[:, 0:1], ALU.mult, r=[acck, rvk], w=[o1k])
                    else:
                        K.tt(rv[:, 0:1], rv[:, 0:1], neg_lam[:], ALU.mult, r=[rvk], w=[rvk])
                        od, odk = odr.next()
                        K.stt(od[:], acc[:, 0:128], rv[:, 0:1], o1[:], ALU.mult, ALU.add, r=[acck, rvk, o1k], w=[odk])
                        jt, jk = jr.next()
                        col = qt * 8 + h
                        K.stt(jt[:], od[:], 1.0, od[:], ALU.mult, ALU.mult, r=[odk], w=[jk, "sso%d" % col],
                              accum_out=ss_o[:, col:col + 1])
                        K.tt(ost[:, qt, :], od[:], gsub08[:], ALU.mult, r=[odk], w=[ok])

                emitS(0)
                emitS(1)
                for u in range(len(units)):
                    if u + 2 < len(units):
                        emitS(u + 2)
                    emitExp(u)
                    emitPV(u)
                K.dma("sync", o_d.rearrange("(t p) c -> p t c", p=128)[:, :, h * 128:(h + 1) * 128], ost[:], r=[ok])
            if debug:
                K.dma("sync", dbg["sso"], ss_o[:], r=["sso%d" % c for c in range(256)])
            S.emit()
        mid.close()
        if upto < 4:
            return nc

        with contextlib.ExitStack() as st:
            wco = sb(st, "wco", [128, 8, 1024], BF16)
            wao = sb(st, "wao", [128, 8, 1024], BF16)
            wout = sb(st, "wout", [128, 8, 1024], BF16)
            for i_, (wt, nm) in enumerate(((wco, "wco"), (wao, "wao"), (wout, "wout"))):
                K.dma("gpsimd", wt[:], w3[i_].rearrange("p (k f) -> p k f", k=8), w=[nm])
            wrt = sb(st, "wrt", [128, 8, 16], F32)
            K.dma("sync", wrt[:], w_router_l.rearrange("p (k f) -> p k f", k=8), w=["wrt"])
            rstd_o = sb(st, "rstd_o", [128, 256], F32)
            K.act(rstd_o[:], ss_o[:], AF.Sqrt, w=["rstd_o"], bias=eps_t[:], scale=1.0 / 128)
            K.recip(rstd_o[:], rstd_o[:], r=["rstd_o"], w=["rstd_o"])
            CH = 256
            NCH = T // CH
            TPC = CH // 128
            yTr = Ring(st, nc, "yT", 2, [128, 8, CH], F32)
            ysqr = Ring(st, nc, "ysq", 1, [128, 8, CH], BF16)
            ybr = Ring(st, nc, "ybf", 1, [128, 8, CH], BF16)
            lnr = Ring(st, nc, "ln", 5, [128, CH], F32)
            tCa = sb(st, "tCa", [128, 8, CH], BF16)
            zTr = Ring(st, nc, "zT", 2, [128, 8, CH], BF16)
            sgcr = Ring(st, nc, "sgc", 2, [128, 8, CH], BF16)
            sgar = Ring(st, nc, "sga", 2, [128, 8, CH], BF16)
            m1r = Ring(st, nc, "m1", 2, [128, 8, CH], BF16)
            otr = Ring(st, nc, "ot", 1, [128, D], BF16)
            onb = sb(st, "onb", [128, 8, 128], BF16)
            oTr = Ring(st, nc, "oT", 2, [128, 8, CH], BF16)
            mixr = Ring(st, nc, "mixin", 2, [128, 8, CH], BF16)
            xr = Ring(st, nc, "x4", 1, [128, D], F32)
            x1r = Ring(st, nc, "x1_", 2, [128, D], F32)
            j4 = sb(st, "j4", [128, D], BF16)
            ssf = sb(st, "ssf", [128, 3 * NT], F32)
            K.memset(ssf[:], 0.0, w=["ssf%d" % i for i in range(NT)])
            hf32r = Ring(st, nc, "hf32", 2, [128, D], F32)
            hfbr = Ring(st, nc, "hfb", 1, [128, D], BF16)
            hfTr = Ring(st, nc, "hfT", 2, [128, 8, 128], F32)
            lgTr = Ring(st, nc, "lgT", 1, [16, 128], F32)
            stz, stm, sto, stx = {}, {}, {}, {}
            woutf, wfk = hf32r.t[0], hf32r.keys[0]
            for kc in range(8):
                K.copy(woutf[:], wout[:, kc, :], r=["wout", wfk], w=[wfk])
                K.tt(wout[:, kc, :], woutf[:], GM, ALU.mult, r=[wfk, "wout"], w=["wout"])

            pre_y, pre_c, pre_a = {}, {}, {}

            def load_y(n):
                t_, k_ = yTr.next()
                K.dma("gpsimd", t_[:], y_d[:, :, n * CH:(n + 1) * CH].rearrange("j p t -> p j t"), w=[k_])
                pre_y[n] = (t_, k_)

            def load_c(n):
                t_, k_ = sgcr.next()
                K.dma("gpsimd", t_[:], sgc_d[:, :, n * CH:(n + 1) * CH].rearrange("j p t -> p j t"), w=[k_])
                pre_c[n] = (t_, k_)

            def load_a(n):
                t_, k_ = sgar.next()
                K.dma("gpsimd", t_[:], sga_d[:, :, n * CH:(n + 1) * CH].rearrange("j p t -> p j t"), w=[k_])
                pre_a[n] = (t_, k_)

            load_y(0)
            load_c(0)
            load_a(0)

            def phaseA(n):
                cs = slice(n * CH, (n + 1) * CH)
                yT, yk = pre_y.pop(n)
                if n + 1 < NCH:
                    load_y(n + 1)
                p1, p1k = K.psnext()
                p2, p2k = K.psnext()
                yb, ybk = ybr.next()
                yq, yqk = ysqr.next()
                K.copy(yb[:], yT[:], r=[yk], w=[ybk])
                K.act(yq[:], yT[:], AF.Square, r=[yk], w=[yqk])
                for j in range(8):
                    K.mm(p1[:, 0:CH], ones_bf[:], yb[:, j, :], j == 0, j == 7, r=[ybk], w=[p1k])
                for j in range(8):
                    K.mm(p2[:, 0:CH], ones_bf[:], yq[:, j, :], j == 0, j == 7, r=[yqk], w=[p2k])
                mean, mk = lnr.next()
                K.ts(mean[:], p1[:, 0:CH], 1.0 / D, ALU.mult, r=[p1k], w=[mk])
                msq, msk = lnr.next()
                K.tt(msq[:], mean[:], mean[:], ALU.mult, r=[mk], w=[msk])
                var, vk_ = lnr.next()
                K.stt(var[:], p2[:, 0:CH], 1.0 / D, msq[:], ALU.mult, ALU.subtract, r=[p2k, msk], w=[vk_])
                sd, sdk = lnr.next()
                K.act(sd[:], var[:], AF.Sqrt, r=[vk_], w=[sdk], bias=eps_t[:], scale=1.0)
                rstd, rk = lnr.next()
                K.recip(rstd[:], sd[:], r=[sdk], w=[rk])
                zT, zk = zTr.next()
                bc = [128, 8, CH]
                K.tt(yT[:], yT[:], mean[:].unsqueeze(1).to_broadcast(bc), ALU.subtract, r=[yk, mk], w=[yk])
                K.tt(yT[:], yT[:], rstd[:].unsqueeze(1).to_broadcast(bc), ALU.mult, r=[yk, rk], w=[yk])
                for j in range(8):
                    K.act(zT[:, j, :], yT[:, j, :], AF.Silu, r=[yk], w=[zk], bias=pv[:, 16 + j:17 + j], scale=pv[:, 8 + j:9 + j])
                stz[n] = (zT, zk)

            def phaseB(n):
                cs = slice(n * CH, (n + 1) * CH)
                zT, zk = stz.pop(n)
                sgc, sgck = pre_c.pop(n)
                if n + 1 < NCH:
                    load_c(n + 1)
                m1, m1k = m1r.next()
                for jb in range(8):
                    pc, pk = K.psnext()
                    for kc in range(8):
                        K.mm(pc[:, 0:CH], wco[:, kc, jb * 128:(jb + 1) * 128], zT[:, kc, :], kc == 0, kc == 7, r=["wco", zk], w=[pk])
                    K.stt(m1[:, jb, :], pc[:, 0:CH], pv[:, 24 + jb:25 + jb], sgc[:, jb, :], ALU.add, ALU.mult,
                          r=[pk, sgck], w=[m1k])
                oT, oTk = oTr.next()
                for tl in range(TPC):
                    tile = n * TPC + tl
                    ot, otk = otr.next()
                    K.dma("gpsimd", ot[:], o_d[tile * 128:(tile + 1) * 128, :], w=[otk])
                    K.tt(onb[:], ot[:].rearrange("p (a b) -> p a b", a=8),
                         rstd_o[:, tile * 8:(tile + 1) * 8].unsqueeze(2).to_broadcast([128, 8, 128]), ALU.mult,
                         r=[otk, "rstd_o"], w=["onb"])
                    ps, pk = K.psnext()
                    tp = ps.bitcast(BF16).rearrange("p (a b) -> p a b", a=8)
                    for kc in range(8):
                        K.tr(tp[:, kc, :], onb[:, kc, :], ident_bf[:], r=["onb"], w=[pk])
                    K.copy(oT[:, :, tl * 128:(tl + 1) * 128], tp, r=[pk], w=[oTk], eng="scalar")
                stm[n] = (m1, m1k)
                sto[n] = (oT, oTk)

            def phaseC(n):
                cs = slice(n * CH, (n + 1) * CH)
                m1, m1k = stm.pop(n)
                oT, oTk = sto.pop(n)
                sga, sgak = pre_a.pop(n)
                if n + 1 < NCH:
                    load_a(n + 1)
                mixin, mxk = mixr.next()
                for jb in range(8):
                    pa, pk = K.psnext()
                    for kc in range(8):
                        K.mm(pa[:, 0:CH], wao[:, kc, jb * 128:(jb + 1) * 128], oT[:, kc, :], kc == 0, kc == 7, r=["wao", oTk], w=[pk])
                    K.tt(tCa[:, jb, :], pa[:, 0:CH], sga[:, jb, :], ALU.mult, r=[pk, sgak], w=["tCa"])
                K.tt(mixin[:], tCa[:], m1[:], ALU.add, r=["tCa", m1k], w=[mxk])
                stx[n] = (mixin, mxk)

            d3q, d4q = [], []

            def phaseD(n):
                mixin, mxk = stx.pop(n)
                for tl in range(TPC):
                    tile = n * TPC + tl
                    xt, xk = xr.next()
                    K.dma("gpsimd", xt[:], x[tile * 128:(tile + 1) * 128, :], w=[xk])
                    x1, x1k = x1r.next()
                    x1ks = [x1k + "_0", x1k + "_1"]
                    for cg in range(2):
                        cgs = slice(cg * 512, (cg + 1) * 512)
                        pm, pk = K.psnext()
                        for kc in range(8):
                            K.mm(pm, mixin[:, kc, tl * 128:(tl + 1) * 128], wout[:, kc, cgs], kc == 0, kc == 7, r=[mxk, "wout"], w=[pk])
                        K.tt(x1[:, cgs], pm, xt[:, cgs], ALU.add, r=[pk, xk], w=[x1ks[cg]])
                    for cg in range(2):
                        K.dma("sync", x2_d[cg][tile * 128:(tile + 1) * 128, :], x1[:, cg * 512:(cg + 1) * 512], r=[x1ks[cg]])
                    sk = "ssf%d" % tile
                    K.act(j4[:], x1[:], AF.Square, r=x1ks, w=["j4", sk], accum_out=ssf[:, tile:tile + 1])
                    K.act(ssf[:, NT + tile:NT + tile + 1], ssf[:, tile:tile + 1], AF.Sqrt, r=[sk], w=[sk], bias=eps_t[:], scale=1.0 / D)
                    K.recip(ssf[:, 2 * NT + tile:2 * NT + tile + 1], ssf[:, NT + tile:NT + tile + 1], r=[sk], w=[sk])
                    hf32, hfk = hf32r.next()
                    K.stt(hf32[:], x1[:], ssf[:, 2 * NT + tile:2 * NT + tile + 1], G2, ALU.mult, ALU.mult, r=x1ks + [sk], w=[hfk])
                    K.tt(hf32[:], hf32[:], SH2, ALU.add, r=[hfk], w=[hfk])
                    hfb, hbk = hfbr.next()
                    K.copy(hfb[:], hf32[:], r=[hfk], w=[hbk])
                    K.dma("sync", hf_d[tile * 128:(tile + 1) * 128, :], hfb[:], r=[hbk])
                    d3q.append((tile, hf32, hfk))
                    if len(d3q) > 1:
                        phaseD3(*d3q.pop(0))
                    if len(d4q) > 1:
                        phaseD4(*d4q.pop(0))

            def phaseD3(tile, hf32, hfk):
                pp, ppk = K.pspair()
                tpf = pp[:].rearrange("p (a b) -> p a b", a=8)
                for kc in range(8):
                    K.tr(tpf[:, kc, :], hf32[:, kc * 128:(kc + 1) * 128], ident_f, r=[hfk], w=ppk)
                hfT, hfTk = hfTr.next()
                K.copy(hfT[:], tpf, r=ppk, w=[hfTk], eng="scalar")
                d4q.append((tile, hfT, hfTk))

            def phaseD4(tile, hfT, hfTk):
                pl, plk = K.psnext()
                for kc in range(8):
                    K.mm(pl[0:16, 0:128], wrt[:, kc, :], hfT[:, kc, :], kc == 0, kc == 7, r=[hfTk, "wrt"], w=[plk])
                lgT, lgTk = lgTr.next()
                K.copy(lgT[:], pl[0:16, 0:128], r=[plk], w=[lgTk], eng="scalar")
                pl2, pl2k = K.psnext()
                K.tr(pl2[:, 0:16], lgT[:], ident_f[0:16, 0:16], r=[lgTk], w=[pl2k])
                K.copy(lg[:, tile, :], pl2[:, 0:16], r=[pl2k], w=["lg%d" % tile])

            for s_ in range(NCH + 3):
                if s_ < NCH:
                    phaseA(s_)
                if 0 <= s_ - 1 < NCH:
                    phaseB(s_ - 1)
                if 0 <= s_ - 2 < NCH:
                    phaseC(s_ - 2)
                if 0 <= s_ - 3 < NCH:
                    phaseD(s_ - 3)
            while d3q:
                phaseD3(*d3q.pop(0))
            while d4q:
                phaseD4(*d4q.pop(0))
            if debug:
                K.dma("sync", dbg["lg"], lg[:].rearrange("p a b -> p (a b)"), r=["lg%d" % t_ for t_ in range(NT)])
            S.emit()
        if upto < 5:
            return nc

        late = contextlib.ExitStack()
        pos = sb(late, "pos", [128, 32, 16], F32)
        vals = sb(late, "vals", [128, 32, 16, 5], BF16)
        selr = Ring(late, nc, "sel", 6, [128, 512], BF16)
        c5r = Ring(late, nc, "c5", 2, [128, 5], F32)
        idxf = sb(late, "idxf", [128, 64], F32)
        g1 = sb(late, "g1", [128, 64], F32)

        sels = {}

        def comp_sel(e, i0, i1):
            for i in range(i0, i1):
                sl_, slk = selr.next()
                K.ts(sl_[:], iota_row, pos[:, i, e:e + 1], ALU.is_equal, r=["pos"], w=[slk])
                sels[(e, i)] = (sl_, slk)

        def comp_steps(e, i0, i1, pa_, pak):
            pacc = [pa_[:, 128 * q_:128 * q_ + 5] for q_ in range(4)]
            for i in range(i0, i1):
                if (e, i) in sels:
                    sl_, slk = sels.pop((e, i))
                else:
                    sl_, slk = selr.next()
                    K.ts(sl_[:], iota_row, pos[:, i, e:e + 1], ALU.is_equal, r=["pos"], w=[slk])
                for s4 in range(4):
                    K.mm(pacc[s4], sl_[:, s4 * 128:(s4 + 1) * 128], vals[:, i, e, :], i == 0 and s4 == 0, i == 31 and s4 == 3,
                         r=[slk, "vals"], w=[pak])
            if i1 < 32:
                return
            for s4 in range(4):
                col = e * 4 + s4
                c5, c5k = c5r.next()
                K.copy(c5[:], pacc[s4], r=[pak], w=[c5k])
                K.stt(idxf[:, col:col + 1], c5[:, 1:2], 128.0, c5[:, 0:1], ALU.mult, ALU.add, r=[c5k], w=["idxf%d" % col])
                K.copy(idx_all[:, col:col + 1], idxf[:, col:col + 1], r=["idxf%d" % col], w=["idx%d" % col])
                K.tt(g1[:, col:col + 1], c5[:, 4:5], c5[:, 3:4], ALU.add, r=[c5k], w=["g1%d" % col])
                K.tt(gate_all[:, col:col + 1], g1[:, col:col + 1], c5[:, 2:3], ALU.add, r=[c5k, "g1%d" % col], w=["gate%d" % col])

        with contextlib.ExitStack() as st:
            def b16(ap):
                return ap.unsqueeze(1).to_broadcast([128, 32, 16])

            def b32(ap):
                return ap.unsqueeze(2).to_broadcast([128, 32, 16])

            mx = sb(st, "mx", [128, 32], F32)
            K.reduce(mx[:], lg[:], ALU.max, w=["mx"])
            ex = sb(st, "ex", [128, 32, 16], F32)
            K.tt(ex[:], lg[:], b32(mx[:]), ALU.subtract, r=["mx"], w=["ex"])
            K.act(ex[:], ex[:], AF.Exp, r=["ex"], w=["ex"])
            sm = sb(st, "sm", [128, 32], F32)
            K.reduce(sm[:], ex[:], ALU.add, r=["ex"], w=["sm"])
            K.recip(sm[:], sm[:], r=["sm"], w=["sm"])
            aff = sb(st, "aff", [128, 32, 16], F32)
            K.tt(aff[:], ex[:], b32(sm[:]), ALU.mult, r=["ex", "sm"], w=["aff"])
            lo = sb(st, "lo", [128, 16], F32)
            K.memset(lo[:], 0.0, w=["lo"])
            cand = sb(st, "cand", [128, 16], F32)
            cmpr = Ring(st, nc, "cmp", 2, [128, 32, 16], BF16)
            cnt = sb(st, "cnt", [128, 16], F32)
            sel_ = sb(st, "sel_", [128, 16], F32)
            for kb in range(1, NBIS + 1):
                s_ = 2.0 ** (-kb)
                K.ts(cand[:], lo[:], s_, ALU.add, r=["lo"], w=["cand"])
                cmp_, ck = cmpr.next()
                K.tt(cmp_[:], aff[:], b16(cand[:]), ALU.is_gt, r=["aff", "cand"], w=[ck])
                pcn, pk = K.psnext()
                K.mm(pcn, ones_bf[:], cmp_[:].rearrange("p a b -> p (a b)"), True, True, r=[ck], w=[pk])
                K.reduce(cnt[:], pcn.rearrange("p (t e) -> p e t", e=16), ALU.add, r=[pk], w=["cnt"])
                K.ts(sel_[:], cnt[:], float(CAP) - 0.5, ALU.is_gt, r=["cnt"], w=["sel_"], s2=s_, op1=ALU.mult)
                K.tt(lo[:], lo[:], sel_[:], ALU.add, r=["lo", "sel_"], w=["lo"])
            maskf = sb(st, "maskf", [128, 32, 16], F32)
            K.tt(maskf[:], aff[:], b16(lo[:]), ALU.is_gt, r=["aff", "lo"], w=["maskf"])
            maskb = sb(st, "maskb", [128, 32, 16], BF16)
            K.copy(maskb[:], maskf[:], r=["maskf"], w=["maskb"])
            ppre, ppk = K.psnext()
            K.mm(ppre, tri_bf[:], maskb[:].rearrange("p a b -> p (a b)"), True, True, r=["maskb"], w=[ppk])
            ptot, ptk = K.psnext()
            K.mm(ptot, ones_bf[:], maskb[:].rearrange("p a b -> p (a b)"), True, True, r=["maskb"], w=[ptk])
            tot = sb(st, "tot", [128, 32, 16], F32)
            K.copy(tot[:].rearrange("p a b -> p (a b)"), ptot, r=[ptk], w=["tot"])
            offs = sb(st, "offs", [128, 32, 16], F32)
            K.memset(offs[:, 0, :], 0.0, w=["offs"])
            for i in range(1, 32):
                K.tt(offs[:, i, :], offs[:, i - 1, :], tot[:, i - 1, :], ALU.add, r=["offs", "tot"], w=["offs"])
            K.tt(pos[:].rearrange("p a b -> p (a b)"), ppre, offs[:].rearrange("p a b -> p (a b)"), ALU.add, r=[ppk, "offs"], w=["pos"])
            K.tt(pos[:], pos[:], maskf[:], ALU.mult, r=["pos", "maskf"], w=["pos"])
            K.ts(pos[:], pos[:], -1.0, ALU.add, r=["pos"], w=["pos"])
            K.copy(vals[:, :, :, 0], pidx.unsqueeze(2).to_broadcast([128, 32, 16]), w=["vals"])
            K.copy(vals[:, :, :, 1], b32(tileidx), r=["vals"], w=["vals"])
            K.copy(vals[:, :, :, 2], aff[:], r=["aff", "vals"], w=["vals"])
            h32 = sb(st, "h32", [128, 32, 16], F32)
            r1 = sb(st, "r1", [128, 32, 16], F32)
            K.copy(h32[:], vals[:, :, :, 2], r=["vals"], w=["h32"])
            K.tt(r1[:], aff[:], h32[:], ALU.subtract, r=["aff", "h32"], w=["r1"])
            K.copy(vals[:, :, :, 3], r1[:], r=["r1", "vals"], w=["vals"])
            K.copy(h32[:], vals[:, :, :, 3], r=["vals", "h32"], w=["h32"])
            K.tt(r1[:], r1[:], h32[:], ALU.subtract, r=["r1", "h32"], w=["r1"])
            K.copy(vals[:, :, :, 4], r1[:], r=["r1", "vals"], w=["vals"])
            NPRE = NE if debug and upto < 6 else 2
            for e in range(NPRE):
                pa_, pak = K.psnext()
                comp_steps(e, 0, 32, pa_, pak)
            if debug:
                K.dma("sync", dbg["aff"], aff[:].rearrange("p a b -> p (a b)"), r=["aff"])
                K.dma("sync", dbg["pos"], pos[:].rearrange("p a b -> p (a b)"), r=["pos"])
                K.dma("sync", dbg["idx"], idx_all[:], r=["idx%d" % c for c in range(64)])
                K.dma("sync", dbg["gate"], gate_all[:], r=["gate%d" % c for c in range(64)])
            S.emit()
        if upto < 6:
            late.close()
            return nc

        with contextlib.ExitStack() as st:
            xsr = Ring(st, nc, "xs", 2, [128, 4, D], BF16)
            xsTr = Ring(st, nc, "xsT", 2, [128, 8, 512], BF16)
            wgr = Ring(st, nc, "wg", 6, [128, 8, 256], BF16)
            wur = Ring(st, nc, "wu", 6, [128, 8, 256], BF16)
            wdr = Ring(st, nc, "wd", 2, [128, NFB, 512], BF16)
            hidr = Ring(st, nc, "hid", 1, [128, NFB, 512], BF16)
            sgtr = Ring(st, nc, "sgt", 2, [128, 512], F32)
            ysgr = Ring(st, nc, "ysg", 3, [128, 512], F32)
            loads = []
            for e in range(NE):
                for c in range(11):
                    loads.append(("g", e, c))
                    loads.append(("u", e, c))
                    if c == 4:
                        loads.append(("d", e, 0))
                    if c == 9:
                        loads.append(("d", e, 1))
            issued = {}
            state = {"i": 0}

            def issue_until(key):
                while key not in issued:
                    kind, e, c = loads[state["i"]]
                    state["i"] += 1
                    if kind == "g":
                        t_, k_ = wgr.next()
                        K.dma("gpsimd", t_[:], wg_l[e, c].rearrange("p (k f) -> p k f", k=8), w=[k_])
                    elif kind == "u":
                        t_, k_ = wur.next()
                        K.dma("gpsimd", t_[:], wu_l[e, c].rearrange("p (k f) -> p k f", k=8), w=[k_])
                    else:
                        t_, k_ = wdr.next()
                        K.dma("gpsimd", t_[:], wd_l[e, c].rearrange("p (k f) -> p k f", k=NFB), w=[k_])
                    issued[(kind, e, c)] = (t_, k_)

            pos_of = {tuple(k_): i_ for i_, k_ in enumerate(loads)}

            def issue_to(idx):
                tgt = min(idx + 1, len(loads))
                while state["i"] < tgt:
                    issue_until(tuple(loads[state["i"]]))

            gathered = {}

            def gather(e):
                xs_, xk_ = xsr.next()
                for s4 in range(4):
                    col = e * 4 + s4
                    S.add("gpsimd", (lambda xs_=xs_, s4=s4, col=col: lambda g: g.indirect_dma_start(
                        out=xs_[:, s4, :], out_offset=None, in_=hf_d,
                        in_offset=bass.IndirectOffsetOnAxis(ap=idx_all[:, col:col + 1], axis=0)))(),
                        r=["idx%d" % col], w=[xk_ + "_%d" % s4], dma=True)
                gathered[e] = (xs_, xk_)

            xsTs = {}

            def transp(e):
                xs_, xk_ = gathered.pop(e)
                xsT, xTk = xsTr.next()
                if debug and e == 0:
                    K.dma("sync", dbg["xs0"], xs_[:].rearrange("p a b -> p (a b)"), r=[xk_ + "_%d" % q_ for q_ in range(4)])
                for s4 in range(4):
                    ps, pk = K.psnext()
                    tp = ps.bitcast(BF16).rearrange("p (a b) -> p a b", a=8)
                    for kc in range(8):
                        K.tr(tp[:, kc, :], xs_[:, s4, kc * 128:(kc + 1) * 128], ident_bf[:], r=[xk_ + "_%d" % s4], w=[pk])
                    K.copy(xsT[:, :, s4 * 128:(s4 + 1) * 128], tp, r=[pk], w=[xTk], eng="scalar" if s4 % 2 else "vector")
                xsTs[e] = (xsT, xTk)

            K.reserved = {7}
            cbank, cbk = K.bank(7)
            gather(0)
            transp(0)
            for e in range(NE):
                if e + 1 < NE:
                    gather(e + 1)
                xsT, xTk = xsTs.pop(e)
                hid, hk = hidr.next()
                for c in range(11):
                    issue_to(pos_of[("u", e, c)] + 10)
                    wgb, wgk = issued[("g", e, c)]
                    wub, wuk = issued[("u", e, c)]
                    for fbi in range(2):
                        fb = c * 2 + fbi
                        pg, pgk = K.psnext()
                        for kc in range(8):
                            K.mm(pg, wgb[:, kc, fbi * 128:(fbi + 1) * 128], xsT[:, kc, :], kc == 0, kc == 7, r=[wgk, xTk], w=[pgk])
                        pu, puk = K.psnext()
                        for kc in range(8):
                            K.mm(pu, wub[:, kc, fbi * 128:(fbi + 1) * 128], xsT[:, kc, :], kc == 0, kc == 7, r=[wuk, xTk], w=[puk])
                        sgt, sgk = sgtr.next()
                        K.act(sgt[:], pg, AF.Silu, r=[pgk], w=[sgk])
                        K.tt(hid[:, fb, :], sgt[:], pu, ALU.mult, r=[sgk, puk], w=[hk])
                    if e + 2 < NE:
                        if c > 0:
                            comp_steps(e + 2, 3 * (c - 1), 3 * c, cbank, cbk)
                        comp_sel(e + 2, 3 * c, min(3 * c + 3, 32))
                if e + 2 < NE:
                    comp_steps(e + 2, 30, 32, cbank, cbk)
                if e + 1 < NE:
                    issue_to(pos_of[("u", e + 1, 4)])
                if debug and e == 0:
                    K.dma("sync", dbg["hid0"], hid[:].rearrange("p a b -> p (a b)"), r=[hk])
                if e + 1 < NE:
                    transp(e + 1)
                for cg in range(2):
                    issue_until(("d", e, cg))
                    wdb, wdk = issued[("d", e, cg)]
                    for s4 in range(4):
                        col = e * 4 + s4
                        py, pyk = K.psnext()
                        for fb in range(NFB):
                            K.mm(py, hid[:, fb, s4 * 128:(s4 + 1) * 128], wdb[:, fb, :], fb == 0, fb == NFB - 1, r=[hk, wdk], w=[pyk])
                        ysg, ysk = ysgr.next()
                        K.stt(ysg[:], py, gate_all[:, col:col + 1], GF[:, cg * 512:(cg + 1) * 512], ALU.mult, ALU.mult, r=[pyk, "gate%d" % col], w=[ysk])
                        if debug and col == 0 and cg == 0:
                            K.dma("sync", dbg["ysg0"], ysg[:], r=[ysk])
                        S.add("gpsimd", (lambda ysg=ysg, cg=cg, col=col: lambda g: g.indirect_dma_start(
                            out=x2_d[cg], out_offset=bass.IndirectOffsetOnAxis(ap=idx_all[:, col:col + 1], axis=0),
                            in_=ysg[:], in_offset=None, compute_op=ALU.add))(),
                            r=[ysk, "idx%d" % col] + ["x2_%d_%d_%d" % (cg, (e + 1) % 2, q_) for q_ in range(4)],
                            w=["x2_%d_%d_%d" % (cg, e % 2, s4)], dma=True)
            S.emit()

        K.reserved = set()
        late.close()
        with contextlib.ExitStack() as st:
            gfin = sb(st, "gfin", [128, D], F32)
            K.dma("sync", gfin[:], gvecs_b[:, 2 * D:3 * D], w=["gfin"])
            xr = Ring(st, nc, "x7", 3, [128, D], F32)
            jr = Ring(st, nc, "j7", 2, [128, D], BF16)
            orr = Ring(st, nc, "o7", 3, [128, D], F32)
            s7 = sb(st, "s7", [128, 3 * NT], F32)
            K.memset(s7[:], 0.0, w=["s7_%d" % i for i in range(NT)])
            for tile in range(NT):
                rows = slice(tile * 128, (tile + 1) * 128)
                xt, xk = xr.next()
                K.dma("gpsimd", xt[:, 0:512], x2_d[0][rows, :], w=[xk + "a"])
                K.dma("gpsimd", xt[:, 512:1024], x2_d[1][rows, :], w=[xk + "b"])
                jt, jk = jr.next()
                sk = "s7_%d" % tile
                K.act(jt[:], xt[:], AF.Square, r=[xk + "a", xk + "b"], w=[jk, sk], accum_out=s7[:, tile:tile + 1])
                K.act(s7[:, NT + tile:NT + tile + 1], s7[:, tile:tile + 1], AF.Sqrt, r=[sk], w=[sk], bias=eps_t[:], scale=1.0 / D)
                K.recip(s7[:, 2 * NT + tile:2 * NT + tile + 1], s7[:, NT + tile:NT + tile + 1], r=[sk], w=[sk])
                ot, ok = orr.next()
                K.stt(ot[:], xt[:], s7[:, 2 * NT + tile:2 * NT + tile + 1], gfin[:], ALU.mult, ALU.mult, r=[xk + "a", xk + "b", sk, "gfin"], w=[ok])
                K.dma("sync", out[rows, :], ot[:], r=[ok])
            S.emit()
    return nc


def _blk(W, nb):
    Kd, N = W.shape
    return np.ascontiguousarray(W.reshape(8, 128, N // nb, nb).transpose(2, 1, 0, 3)).reshape(N // nb, 128, 8 * nb)


def _pcol(v):
    return np.ascontiguousarray(v.reshape(8, 128).T)


def _rope_tables():
    inv = (10000.0 ** (-np.arange(16, dtype=np.float32) / 16)).astype(np.float32)
    t = np.arange(T)
    row = (t // 64).astype(np.float32)
    col = (t % 64).astype(np.float32)
    C = np.zeros((128, T), np.float32)
    Sg = np.zeros((128, T), np.float32)
    for p in range(128):
        d = p % 64
        posv = row if d < 32 else col
        dd = d % 32
        j = dd % 16
        half = dd // 16
        ang = (posv * inv[j]).astype(np.float32)
        C[p] = np.cos(ang)
        Sg[p] = np.sin(ang) * (-1.0 if half == 0 else 1.0)
    return C, Sg


def _rot_perm():
    perm = np.zeros(1024, np.int64)
    for c in range(1024):
        base = (c // 64) * 64
        d = c % 64
        dd = d % 32
        half = dd // 16
        perm[c] = base + (d + 16 if half == 0 else d - 16)
    return perm


def prep_shared(inp, upto=99):
    f = np.float32
    sh = {}
    w_ada = np.asarray(inp["w_ada"], f)[0]
    sh["w_ada_l"] = _blk(w_ada, 512)
    sh["bada_b"] = np.ascontiguousarray(np.broadcast_to(np.asarray(inp["b_ada"], f)[0][None, :], (128, 6 * D)))
    gv = np.concatenate([np.asarray(inp["g_norm_mix"], f)[0], np.asarray(inp["g_norm_ffn"], f)[0], np.asarray(inp["g_final"], f)])
    sh["gvecs_b"] = np.ascontiguousarray(np.broadcast_to(gv[None, :], (128, 3 * D)))
    wdw = np.asarray(inp["w_dw"], f)[0]
    wdw_l = np.ascontiguousarray(wdw.T.reshape(8, 128, 31).transpose(1, 0, 2)).reshape(128, 8 * 31)
    sh["pvecs"] = np.ascontiguousarray(np.concatenate([
        _pcol(np.asarray(inp["b_dw"], f)[0]), _pcol(np.asarray(inp["ln_g_conv"], f)[0]),
        _pcol(np.asarray(inp["ln_b_conv"], f)[0]), _pcol(np.asarray(inp["b_conv_out"], f)[0]), wdw_l], axis=1))
    lam = np.concatenate([np.asarray(inp[k], f)[0] for k in ("lambda_q1", "lambda_k1", "lambda_q2", "lambda_k2")])
    sh["lamv_b"] = np.ascontiguousarray(np.broadcast_to(lam[None, :], (128, 256)))
    sh["gsub_b"] = np.ascontiguousarray(np.broadcast_to(np.asarray(inp["g_subln"], f)[0][None, :], (128, 128)))
    cf = np.zeros((128, 801), f)
    cf[:, 0:128] = np.eye(128, dtype=f)
    cf[:, 128:640] = np.arange(512, dtype=f)[None, :]
    cf[:, 640:768] = np.triu(np.ones((128, 128), f))
    cf[:, 768] = np.arange(128, dtype=f)
    cf[:, 769:801] = np.arange(32, dtype=f)[None, :]
    sh["constf"] = cf
    C, Sg = _rope_tables()
    sh["ropeC"], sh["ropeS"] = C, Sg
    w_in = np.asarray(inp["w_in"], f)[0]
    a, b, q, k, v, gc, ga = [w_in[:, i * 1024:(i + 1) * 1024] for i in range(7)]
    perm = _rot_perm()
    qr, kr = q[:, perm], k[:, perm]
    blocks = []
    ab, bb, gcb, gab = _blk(a, 128), _blk(b, 128), _blk(gc, 128), _blk(ga, 128)
    for j in range(8):
        blocks += [ab[j], bb[j], gcb[j], gab[j]]
    qb, qrb, kb, krb, vb = _blk(q, 128), _blk(qr, 128), _blk(k, 128), _blk(kr, 128), _blk(v, 128)
    for h in range(8):
        blocks += [qb[h], qrb[h], kb[h], krb[h], vb[h]]
    sh["w_fm"] = np.ascontiguousarray(np.stack(blocks))

    def plain(W):
        return np.ascontiguousarray(W.reshape(8, 128, W.shape[1]).transpose(1, 0, 2)).reshape(128, 8 * W.shape[1])

    sh["w3"] = np.ascontiguousarray(np.stack([plain(np.asarray(inp["w_conv_out"], f)[0]),
                                              plain(np.asarray(inp["w_attn_out"], f)[0]),
                                              plain(np.asarray(inp["w_out"], f)[0])]))
    sh["w_router_l"] = plain(np.asarray(inp["w_router"], f)[0])
    if upto >= 6:
        wg = np.asarray(inp["w_expert_gate"], f)[0]
        wu = np.asarray(inp["w_expert_up"], f)[0]
        wd = np.asarray(inp["w_expert_down"], f)[0]

        def gl(W):
            return np.ascontiguousarray(W.reshape(NE, 8, 128, 11, 256).transpose(0, 3, 2, 1, 4)).reshape(NE, 11, 128, 2048)

        sh["wg_l"] = gl(wg)
        sh["wu_l"] = gl(wu)
        sh["wd_l"] = np.ascontiguousarray(wd.reshape(NE, NFB, 128, 2, 512).transpose(0, 3, 2, 1, 4)).reshape(NE, 2, 128, NFB * 512)
    return sh


def prep_core(inp, b):
    f = np.float32
    cv = np.concatenate([_pcol(np.asarray(inp["c"], f)[b]), _pcol(np.asarray(inp["c_ctx"], f))], axis=1)
    return {"x": np.ascontiguousarray(np.asarray(inp["x"], f)[b]),
            "ctx": np.ascontiguousarray(np.asarray(inp["ctx"], f)[b]),
            "cvec": np.ascontiguousarray(cv)}


_CACHE = {}


def kernel(**inputs):
    if "nc" not in _CACHE:
        _CACHE["nc"] = build()
    nc = _CACHE["nc"]
    sh = prep_shared(inputs)
    in_maps = []
    for b in range(8):
        m = dict(sh)
        m.update(prep_core(inputs, b))
        in_maps.append(m)
    res = run_bass_kernel_spmd(nc, in_maps, core_ids=list(range(8)))
    return np.stack([np.asarray(r["out"], np.float32) for r in res.results], axis=0)
```
